# Optimizing a Trainium2 kernel written in Bass

```python
import math, functools
import jax, jax.numpy as jnp
from jax import lax
import numpy as np

D_MODEL = 1024
BATCH = 8
SEQ = 2048
DEPTH = 4

CHUNK = 64
N_MIXERS = 2
N_MLSTM_LAYERS = (DEPTH + 1) // 2
N_CONV_LAYERS = DEPTH // 2

M_HEADS = 4
M_DK = 128
M_DV = 256
M_QK = M_HEADS * M_DK
M_V = M_HEADS * M_DV
M_PROJ = 2 * M_QK + 2 * M_V + 2 * M_HEADS

CONV_K = 3

N_GROUPS = 4
E_PER_GROUP = 8
N_EXPERTS = N_GROUPS * E_PER_GROUP
TOP_K = 2
D_FF = 512
ROW_BLOCK = 128

EPS = 1e-6

kernel_name = "hybrid_mlstm_shortconv_hmoe_adaln"


def rmsnorm(x, g):
    xf = x.astype(jnp.float32)
    y = xf * lax.rsqrt(jnp.mean(xf * xf, axis=-1, keepdims=True) + EPS)
    return (y * g.astype(jnp.float32)).astype(x.dtype)


def adaln(c, w_ada, b_ada):
    mod = jax.nn.silu(c) @ w_ada + b_ada
    shift, scale, gate = jnp.split(mod, 3, axis=-1)
    return shift[:, None, :], scale[:, None, :], gate[:, None, :]


def mlstm_mixer(h, w_in, b_gates, norm_g, w_out):
    B, S, _ = h.shape
    nc = S // CHUNK
    proj = h @ w_in
    q, k, v, o, ig, fg = jnp.split(
        proj, [M_QK, 2 * M_QK, 2 * M_QK + M_V, 2 * M_QK + 2 * M_V,
               2 * M_QK + 2 * M_V + M_HEADS], axis=-1)
    f32 = jnp.float32

    def heads_to_chunks(t, d):
        return t.astype(f32).reshape(B, nc, CHUNK, M_HEADS, d).transpose(1, 0, 3, 2, 4)

    def gates_to_chunks(t):
        return t.reshape(B, nc, CHUNK, M_HEADS).transpose(1, 0, 3, 2)

    qc = heads_to_chunks(q, M_DK) * (M_DK ** -0.5)
    kc = heads_to_chunks(k, M_DK)
    vc = heads_to_chunks(v, M_DV)
    b_g = b_gates.astype(f32)
    i_pre = gates_to_chunks(ig.astype(f32) + b_g[:M_HEADS])
    log_f = gates_to_chunks(jax.nn.log_sigmoid(fg.astype(f32) + b_g[M_HEADS:]))
    causal = jnp.tril(jnp.ones((CHUNK, CHUNK), dtype=bool))

    def step(carry, xs):
        C, n, m = carry
        q_, k_, v_, i_, lf_ = xs
        bcum = jnp.cumsum(lf_, axis=-1)
        dlog = bcum[..., :, None] - bcum[..., None, :] + i_[..., None, :]
        dlog = jnp.where(causal, dlog, -jnp.inf)
        inter_log = bcum + m[..., None]
        m_t = jnp.maximum(inter_log, jnp.max(dlog, axis=-1))
        w_intra = jnp.exp(dlog - m_t[..., None])
        w_inter = jnp.exp(inter_log - m_t)
        scores = jnp.einsum('bhtd,bhsd->bhts', q_, k_) * w_intra
        num = (jnp.einsum('bhts,bhsv->bhtv', scores, v_)
               + w_inter[..., None] * jnp.einsum('bhvd,bhtd->bhtv', C, q_))
        den = scores.sum(-1) + w_inter * jnp.einsum('bhd,bhtd->bht', n, q_)
        h_out = num / jnp.maximum(jnp.abs(den), jnp.exp(-m_t))[..., None]
        b_last = bcum[..., -1]
        log_src = b_last[..., None] - bcum + i_
        m_new = jnp.maximum(b_last + m, jnp.max(log_src, axis=-1))
        w_src = jnp.exp(log_src - m_new[..., None])
        decay = jnp.exp(b_last + m - m_new)
        C_new = decay[..., None, None] * C + jnp.einsum('bhsv,bhsd->bhvd', v_ * w_src[..., None], k_)
        n_new = decay[..., None] * n + jnp.einsum('bhs,bhsd->bhd', w_src, k_)
        return (C_new, n_new, m_new), h_out

    init = (jnp.zeros((B, M_HEADS, M_DV, M_DK), f32),
            jnp.zeros((B, M_HEADS, M_DK), f32),
            jnp.zeros((B, M_HEADS), f32))
    _, hc = lax.scan(step, init, (qc, kc, vc, i_pre, log_f))
    hs = hc.transpose(1, 0, 3, 2, 4).reshape(B, S, M_HEADS, M_DV)
    hs = hs * lax.rsqrt(jnp.mean(hs * hs, axis=-1, keepdims=True) + EPS)
    hs = hs.reshape(B, S, M_V) * norm_g.astype(f32) * jax.nn.sigmoid(o.astype(f32))
    return hs.astype(h.dtype) @ w_out


def short_conv_mixer(h, w_in, conv_w, w_out):
    b_gate, c_gate, xb = jnp.split(h @ w_in, 3, axis=-1)
    u = c_gate * xb
    y = lax.conv_general_dilated(
        u, conv_w[:, None, :].astype(u.dtype), window_strides=(1,),
        padding=[(CONV_K - 1, 0)], dimension_numbers=('NWC', 'WIO', 'NWC'),
        feature_group_count=D_MODEL)
    return (b_gate * y) @ w_out


def hier_moe(h, w_group, b_group, w_expert, b_expert, w_gate, w_up, w_down):
    B, S, D = h.shape
    N = B * S
    xt = h.reshape(N, D)
    f32 = jnp.float32
    g_logits = (xt @ w_group).astype(f32) + b_group.astype(f32)
    g_prob = jax.nn.softmax(g_logits, axis=-1)
    g_sel = jnp.argmax(g_logits, axis=-1).astype(jnp.int32)
    p_sel = jnp.take_along_axis(g_prob, g_sel[:, None], axis=-1)[:, 0]
    e_logits = ((xt @ w_expert).astype(f32) + b_expert.astype(f32)).reshape(N, N_GROUPS, E_PER_GROUP)
    e_sel = jnp.take_along_axis(e_logits, g_sel[:, None, None], axis=1)[:, 0]
    top_l, top_i = lax.top_k(e_sel, TOP_K)
    combine = jax.nn.softmax(top_l, axis=-1) * p_sel[:, None]
    eid = g_sel[:, None] * E_PER_GROUP + top_i.astype(jnp.int32)

    A = N * TOP_K
    flat_e = eid.reshape(A)
    flat_w = combine.reshape(A)
    flat_tok = jnp.repeat(jnp.arange(N, dtype=jnp.int32), TOP_K)
    order = jnp.argsort(flat_e)
    se, stok, sw = flat_e[order], flat_tok[order], flat_w[order]
    counts = jax.ops.segment_sum(jnp.ones((A,), jnp.int32), flat_e, num_segments=N_EXPERTS)
    starts = jnp.cumsum(counts) - counts
    pcounts = (counts + ROW_BLOCK - 1) // ROW_BLOCK * ROW_BLOCK
    pends = jnp.cumsum(pcounts)
    pstarts = pends - pcounts
    dest = pstarts[se] + (jnp.arange(A, dtype=jnp.int32) - starts[se])
    n_blocks = (A + N_EXPERTS * (ROW_BLOCK - 1) + ROW_BLOCK - 1) // ROW_BLOCK
    P = n_blocks * ROW_BLOCK
    row_tok = jnp.zeros((P,), jnp.int32).at[dest].set(stok)
    row_w = jnp.zeros((P,), f32).at[dest].set(sw)
    blk_e = jnp.clip(jnp.searchsorted(pends, jnp.arange(n_blocks, dtype=jnp.int32) * ROW_BLOCK,
                                      side='right'), 0, N_EXPERTS - 1).astype(jnp.int32)
    xr = xt[row_tok].reshape(n_blocks, ROW_BLOCK, D)

    def expert_block(args):
        xb, e = args
        return (jax.nn.silu(xb @ w_gate[e]) * (xb @ w_up[e])) @ w_down[e]

    yr = lax.map(expert_block, (xr, blk_e)).reshape(P, D)
    out = jax.ops.segment_sum(yr.astype(f32) * row_w[:, None], row_tok, num_segments=N)
    return out.astype(h.dtype).reshape(B, S, D)


def setup_inputs(seed: int = 0) -> dict:
    key = jax.random.key(seed)
    ks = jax.random.split(key, 24)
    f32 = jnp.float32

    def nrm(k, shape, fan_in, mult=1.0):
        return jax.random.normal(k, shape, f32) * (mult * fan_in ** -0.5)

    D = D_MODEL
    x = jax.random.normal(ks[0], (BATCH, SEQ, D), f32)
    c = jax.random.normal(ks[1], (BATCH, D), f32)
    ada_w = nrm(ks[2], (DEPTH, 2, D, 3 * D), D, 0.5)
    ada_b = 0.02 * jax.random.normal(ks[3], (DEPTH, 2, 3 * D), f32)
    norm_g = 1.0 + 0.02 * jax.random.normal(ks[4], (DEPTH, 2, D), f32)
    final_g = 1.0 + 0.02 * jax.random.normal(ks[5], (D,), f32)
    m_w_in = nrm(ks[6], (N_MLSTM_LAYERS, D, M_PROJ), D)
    m_b_gates = jnp.concatenate([
        0.1 * jax.random.normal(ks[7], (N_MLSTM_LAYERS, M_HEADS), f32),
        3.0 + 0.1 * jax.random.normal(ks[8], (N_MLSTM_LAYERS, M_HEADS), f32)], axis=-1)
    m_norm_g = 1.0 + 0.02 * jax.random.normal(ks[9], (N_MLSTM_LAYERS, M_V), f32)
    m_w_out = nrm(ks[10], (N_MLSTM_LAYERS, M_V, D), M_V)
    s_w_in = nrm(ks[11], (N_CONV_LAYERS, D, 3 * D), D)
    s_conv_w = nrm(ks[12], (N_CONV_LAYERS, CONV_K, D), CONV_K)
    s_w_out = nrm(ks[13], (N_CONV_LAYERS, D, D), D)
    r_w_group = nrm(ks[14], (DEPTH, D, N_GROUPS), D)
    r_b_group = 0.01 * jax.random.normal(ks[15], (DEPTH, N_GROUPS), f32)
    r_w_expert = nrm(ks[16], (DEPTH, D, N_EXPERTS), D)
    r_b_expert = 0.01 * jax.random.normal(ks[17], (DEPTH, N_EXPERTS), f32)
    e_w_gate = nrm(ks[18], (DEPTH, N_EXPERTS, D, D_FF), D)
    e_w_up = nrm(ks[19], (DEPTH, N_EXPERTS, D, D_FF), D)
    e_w_down = nrm(ks[20], (DEPTH, N_EXPERTS, D_FF, D), D_FF)
    return {"x": x, "c": c, "ada_w": ada_w, "ada_b": ada_b, "norm_g": norm_g,
            "final_g": final_g, "m_w_in": m_w_in, "m_b_gates": m_b_gates,
            "m_norm_g": m_norm_g, "m_w_out": m_w_out, "s_w_in": s_w_in,
            "s_conv_w": s_conv_w, "s_w_out": s_w_out, "r_w_group": r_w_group,
            "r_b_group": r_b_group, "r_w_expert": r_w_expert, "r_b_expert": r_b_expert,
            "e_w_gate": e_w_gate, "e_w_up": e_w_up, "e_w_down": e_w_down}


def reference(x, c, ada_w, ada_b, norm_g, final_g, m_w_in, m_b_gates, m_norm_g,
              m_w_out, s_w_in, s_conv_w, s_w_out, r_w_group, r_b_group,
              r_w_expert, r_b_expert, e_w_gate, e_w_up, e_w_down):
    for i in range(DEPTH):
        shift, scale, gate = adaln(c, ada_w[i, 0], ada_b[i, 0])
        h = rmsnorm(x, norm_g[i, 0]) * (1.0 + scale) + shift
        j = i // N_MIXERS
        if i % N_MIXERS == 0:
            y = mlstm_mixer(h, m_w_in[j], m_b_gates[j], m_norm_g[j], m_w_out[j])
        else:
            y = short_conv_mixer(h, s_w_in[j], s_conv_w[j], s_w_out[j])
        x = x + gate * y
        shift, scale, gate = adaln(c, ada_w[i, 1], ada_b[i, 1])
        h = rmsnorm(x, norm_g[i, 1]) * (1.0 + scale) + shift
        y = hier_moe(h, r_w_group[i], r_b_group[i], r_w_expert[i], r_b_expert[i],
                     e_w_gate[i], e_w_up[i], e_w_down[i])
        x = x + gate * y
    return rmsnorm(x, final_g)
```

```python
import numpy as np
import concourse.bass as bass
import concourse.mybir as mybir
from concourse.bass_utils import run_bass_kernel_spmd

F32 = mybir.dt.float32
BF16 = mybir.dt.bfloat16
I32 = mybir.dt.int32
U32 = mybir.dt.uint32
AF = mybir.ActivationFunctionType
ALU = mybir.AluOpType
AX = mybir.AxisListType

S = 2048
D = 1024
NT = 16
DEPTH = 4
NH = 4
DVA = 258
NVA = 257
NE = 32
CAP = 512
NJ = CAP // 128
EPS = 1e-6
ENGS = ("pe", "act", "dve", "pool", "sp")


class Prog:
    def __init__(self, nc):
        self.nc = nc
        self.ops = {e: [] for e in ENGS}
        self.last_w = {}
        self.readers = {}
        self.dma_cnt = {}
        self.pending = {e: {} for e in ENGS}
        self.last_compute = {e: None for e in ENGS}

    @staticmethod
    def _merge(dst, ev, kind):
        k = ev[:2]
        old = dst.get(k)
        if old is None:
            old = [None, None]
            dst[k] = old
        if old[0] is None or old[0][2] < ev[2]:
            old[0] = ev
        if kind != "war" and (old[1] is None or old[1][2] < ev[2]):
            old[1] = ev

    def add(self, eng, fn, r=(), w=(), dma=None):
        idx = len(self.ops[eng])
        deps = {}
        for k, v in self.pending[eng].items():
            deps[k] = list(v)
        self.pending[eng] = {}
        for res in r:
            ev = self.last_w.get(res)
            if ev is not None:
                self._merge(deps, ev, "raw")
        for res in w:
            ev = self.last_w.get(res)
            if ev is not None:
                self._merge(deps, ev, "waw")
            for ev2 in self.readers.get(res, {}).values():
                self._merge(deps, ev2, "war")
        if dma is not None:
            self.dma_cnt[dma] = self.dma_cnt.get(dma, 0) + 16
            myev = ("d", dma, self.dma_cnt[dma])
        else:
            myev = ("c", eng, idx)
            self.last_compute[eng] = myev
        need = []
        for ev_any, ev_nw in deps.values():
            if ev_any[0] == "c" and ev_any[1] == eng and dma is None and eng == "pe":
                continue
            need.append(ev_any)
        self.ops[eng].append(dict(fn=fn, deps=need, ev=myev, dma=dma))
        for res in r:
            self.readers.setdefault(res, {})[myev[:2]] = myev
        for res in w:
            self.last_w[res] = myev
            self.readers[res] = {}
        return myev

    def barrier(self):
        evs = []
        for e in ENGS:
            if self.last_compute[e] is not None:
                evs.append(self.last_compute[e])
        for k, c in self.dma_cnt.items():
            evs.append(("d", k, c))
        for e in ENGS:
            for ev in evs:
                self._merge(self.pending[e], ev, "raw")
        self.last_w = {}
        self.readers = {}

    def emit(self, handles, eng_sems, dma_sems):
        sig = {e: set() for e in ENGS}
        for e in ENGS:
            for op in self.ops[e]:
                for ev in op["deps"]:
                    if ev[0] == "c":
                        sig[ev[1]].add(ev[2])
        rank = {}
        for e in ENGS:
            n = 0
            rank[e] = {}
            for i in range(len(self.ops[e])):
                if i in sig[e]:
                    n += 1
                    rank[e][i] = n

        def run(eng):
            def body(h):
                waited = {}
                for i, op in enumerate(self.ops[eng]):
                    for ev in op["deps"]:
                        if ev[0] == "c":
                            key = ("c", ev[1])
                            val = rank[ev[1]][ev[2]]
                            sem = eng_sems[ev[1]]
                        else:
                            key = ("d", ev[1])
                            val = ev[2]
                            sem = dma_sems[ev[1]]
                        if waited.get(key, 0) >= val:
                            continue
                        h.wait_ge(sem, val)
                        waited[key] = val
                    inst = op["fn"](h)
                    if op["dma"] is not None:
                        inst.then_inc(dma_sems[op["dma"]], 16)
                    elif i in sig[eng]:
                        inst.then_inc(eng_sems[eng], 1)
                if eng == "sp":
                    for k, c in self.dma_cnt.items():
                        if waited.get(("d", k), 0) < c:
                            h.wait_ge(dma_sems[k], c)
            return body
        return {e: run(e) for e in ENGS}


class Arena:
    def __init__(self, t, nbytes):
        self.t = t
        self.n = nbytes
        self.off = 0

    def reset(self):
        self.off = 0

    def alloc(self, shape, dt):
        esz = 4 if dt in (F32, I32, U32) else 2
        per = 1
        for s in shape[1:]:
            per *= s
        nb = (per * esz + 63) // 64 * 64
        assert self.off + nb <= self.n, ("arena overflow", self.off, nb, self.n)
        v = self.t[:, self.off // 4:(self.off + nb) // 4]
        self.off += nb
        if dt != F32:
            v = v.bitcast(dt)
        v = v[0:shape[0], 0:per]
        if len(shape) == 3:
            v = v.rearrange("p (a b) -> p a b", a=shape[1])
        elif len(shape) == 4:
            v = v.rearrange("p (a b c) -> p a b c", a=shape[1], b=shape[2])
        return v


def build_program(n_sub=2 * DEPTH, do_final=True):
    nc = bass.Bass("TRN2", target_bir_lowering=False)

    def din(name, shape, dt=F32):
        return nc.dram_tensor(name, shape, dt, kind="ExternalInput").ap()

    x_d = din("x", [S, D])
    c_d = din("c", [128, 8])
    adaw_d = din("ada_w", [2 * DEPTH, D, 3 * D])
    adab_d = din("ada_b", [2 * DEPTH, 3 * D])
    ng_d = din("norm_g", [2 * DEPTH, D])
    fg_d = din("final_g", [1, D])
    mwin_d = din("m_w_in", [2, D, 3080])
    mbg_d = din("m_b_gates", [2, 8])
    mng_d = din("m_norm_g", [2, D])
    mwout_d = din("m_w_out", [2, D, D])
    swin_d = din("s_w_in", [2, D, 3 * D])
    scw_d = din("s_conv_w", [2, 128, 24])
    swout_d = din("s_w_out", [2, D, D])
    rw_d = din("r_w", [DEPTH, D, 36])
    rb_d = din("r_b", [DEPTH, 36])
    ewg_d = din("e_w_gate", [DEPTH * NE, D, 512])
    ewu_d = din("e_w_up", [DEPTH * NE, D, 512])
    ewd_d = din("e_w_down", [DEPTH * NE, 512, D])
    y_d = nc.dram_tensor("y", [S, D], F32, kind="ExternalOutput").ap()
    Xd = nc.dram_tensor("xd_scr", [NE * CAP, D], BF16, kind="Internal").ap()
    Yd = nc.dram_tensor("yd_scr", [NE * CAP, D], BF16, kind="Internal").ap()

    ARENA_BYTES = 116 * 1024
    from contextlib import ExitStack
    with ExitStack() as es:
        def sb(name, shape, dt=F32):
            return es.enter_context(nc.sbuf_tensor(name, shape, dt))

        xres = sb("xres", [128, NT, D])
        modA = sb("modA", [128, D])
        modB = sb("modB", [128, D])
        modG = sb("modG", [128, D])
        ident_f = sb("ident_f", [128, 128])
        ident_b = sb("ident_b", [128, 128], BF16)
        maskT = sb("maskT", [128, 128])
        Lt = sb("Lt", [128, 128])
        ones_f = sb("ones_f", [128, 128])
        TT = sb("TT", [64, 64])
        iota_i = sb("iota_i", [128, 32], I32)
        iota_f = sb("iota_f", [128, 32])
        cs = sb("cs", [128, 8])
        sc = sb("sc", [128, 8])
        junk = sb("junk", [128, D], BF16)
        tmpf = sb("tmpf", [128, D])
        hbf = sb("hbf", [128, D], BF16)
        st = sb("st", [128, 16])
        RSTD = sb("RSTD", [128, NT])
        SSt = sb("SSt", [128, NT])
        ntmp = sb("ntmp", [128, D])
        arena_t = sb("arena", [128, ARENA_BYTES // 4])
        ar = Arena(arena_t, ARENA_BYTES)
        psall = es.enter_context(nc.psum_tensor("psall", [128, 8, 512], F32))
        ps = [psall[:, i, :] for i in range(8)]
        psb = [psall[:, i, :].bitcast(BF16).rearrange("p (k m) -> p k m", k=8) for i in range(8)]
        negh = sb("negh", [128, 4])

        P = Prog(nc)

        def mm(out, lhsT, rhs, start, stop, r, w):
            P.add("pe", lambda e: e.matmul(out, lhsT=lhsT, rhs=rhs, start=start, stop=stop), r=r, w=w)

        def tr(out, in_, ident, r, w):
            P.add("pe", lambda e: e.transpose(out=out, in_=in_, identity=ident), r=r, w=w)

        def act(out, in_, func, r, w, scale=1.0, bias=0.0, accum=None, eng="act"):
            P.add(eng, lambda e: e.activation(out=out, in_=in_, func=func, bias=bias, scale=scale,
                                              accum_out=accum), r=r, w=w)

        def tt(eng, out, in0, in1, op, r, w):
            P.add(eng, lambda e: e.tensor_tensor(out=out, in0=in0, in1=in1, op=op), r=r, w=w)

        def ts(eng, out, in0, s1, op0, r, w, s2=None, op1=None):
            if op1 is None:
                P.add(eng, lambda e: e.tensor_scalar(out=out, in0=in0, scalar1=s1, scalar2=None, op0=op0), r=r, w=w)
            else:
                P.add(eng, lambda e: e.tensor_scalar(out=out, in0=in0, scalar1=s1, scalar2=s2, op0=op0, op1=op1),
                      r=r, w=w)

        def stt(out, in0, scalar, in1, op0, op1, r, w):
            P.add("dve", lambda e: e.scalar_tensor_tensor(out=out, in0=in0, scalar=scalar, in1=in1,
                                                          op0=op0, op1=op1), r=r, w=w)

        def cp(eng, out, in_, r, w):
            if eng == "act":
                P.add("act", lambda e: e.copy(out=out, in_=in_), r=r, w=w)
            else:
                P.add(eng, lambda e: e.tensor_copy(out=out, in_=in_), r=r, w=w)

        def recip(out, in_, r, w):
            P.add("dve", lambda e: e.reciprocal(out=out, in_=in_), r=r, w=w)

        def dma(q, out, in_, key, r, w):
            P.add(q, lambda e: e.dma_start(out=out, in_=in_), r=r, w=w, dma=key)

        def memset(eng, ap, val, w):
            P.add(eng, lambda e: e.memset(ap, val), r=(), w=w)

        def compute_rstd():
            for t in range(NT):
                act(junk[:], xres[:, t, :], AF.Square, [("x", t)], ["junk", "SSt"], accum=SSt[:, t:t + 1])
            ts("dve", SSt[:], SSt[:], 1.0 / D, ALU.mult, ["SSt"], ["SSt"], s2=EPS, op1=ALU.add)
            act(SSt[:], SSt[:], AF.Sqrt, ["SSt"], ["SSt"])
            recip(RSTD[:], SSt[:], ["SSt"], ["RSTD"])

        def adaln_issue(sub, g_src, base=0):
            ar.off = base
            bufs = dict(
                SCB=ar.alloc([128, 8, 128], BF16),
                WA=[ar.alloc([128, 8, D], BF16) for _ in range(3)],
                adab=ar.alloc([128, 3 * D], F32),
                gbc=ar.alloc([128, D], F32),
            )
            for w3 in range(3):
                dma("pool", bufs["WA"][w3],
                    adaw_d[sub, :, w3 * D:(w3 + 1) * D].rearrange("(k p) n -> p k n", p=128),
                    f"wa{w3}", r=(), w=[("WA", w3)])
            dma("sp", bufs["adab"], adab_d[sub, :].partition_broadcast(128), "adab", r=(), w=["adab"])
            dma("sp", bufs["gbc"], g_src.partition_broadcast(128), "gbc", r=(), w=["gbc"])
            return bufs

        def adaln_finish(bufs):
            SCB, WA, adab, gbc = bufs["SCB"], bufs["WA"], bufs["adab"], bufs["gbc"]
            cp("dve", SCB, sc[:].unsqueeze(2).to_broadcast([128, 8, 128]), ["sc"], ["SCB"])
            compute_rstd()
            for blk in range(6):
                b = blk % 2
                which = blk // 2
                hcol = (blk % 2) * 512
                for k in range(8):
                    mm(ps[b], SCB[:, k, :], WA[which][:, k, hcol:hcol + 512], k == 0, k == 7,
                       r=["SCB", ("WA", which)], w=[("ps", b)])
                half = slice(hcol, hcol + 512)
                cols = slice(blk * 512, blk * 512 + 512)
                if which == 0:
                    tt("dve", modB[:, half], ps[b], adab[:, cols], ALU.add, [("ps", b), "adab"], ["modB"])
                elif which == 1:
                    tt("dve", tmpf[:, half], ps[b], adab[:, cols], ALU.add, [("ps", b), "adab"], ["tmpf"])
                    stt(modA[:, half], tmpf[:, half], 1.0, gbc[:, half], ALU.add, ALU.mult,
                        ["tmpf", "gbc"], ["modA"])
                else:
                    tt("dve", modG[:, half], ps[b], adab[:, cols], ALU.add, [("ps", b), "adab"], ["modG"])
            P.barrier()

        def adaln(sub, g_src):
            adaln_finish(adaln_issue(sub, g_src))

        def norm_tile(t, out_bf=None, out_f=None, shift=True, res_f="hf", res_bf="hbf"):
            xr = ("x", t)
            if shift:
                stt(ntmp[:], xres[:, t, :], RSTD[:, t:t + 1], modA[:], ALU.mult, ALU.mult,
                    [xr, "RSTD", "modA"], ["ntmp"])
                if out_f is not None:
                    tt("dve", out_f, ntmp[:], modB[:], ALU.add, ["ntmp", "modB"], [res_f])
                    if out_bf is not None:
                        cp("act", out_bf, out_f, [res_f], [res_bf])
                else:
                    tt("dve", out_bf, ntmp[:], modB[:], ALU.add, ["ntmp", "modB"], [res_bf])
            else:
                stt(out_f, xres[:, t, :], RSTD[:, t:t + 1], modA[:], ALU.mult, ALU.mult,
                    [xr, "RSTD", "modA"], [res_f])

        def transpose_tile_bf(src, src_res, dst, dst_res, bank):
            for k in range(8):
                tr(psb[bank][:, k, :], src[:, k * 128:(k + 1) * 128], ident_b[:], [src_res], [("ps", bank)])
            cp("act", dst, psb[bank], [("ps", bank)], [dst_res])

        def add_to_x(t, half, psum_ap, psres):
            hs = slice(half * 512, half * 512 + 512)
            tt("dve", tmpf[:, hs], psum_ap, modG[:, hs], ALU.mult, [psres, "modG"], ["tmpf"])
            tt("pool", xres[:, t, hs], xres[:, t, hs], tmpf[:, hs], ALU.add, [("x", t), "tmpf"], [("x", t)])

        def mlstm(j):
            ar.reset()
            Wq = ar.alloc([128, 8, 512], BF16)
            Wk = ar.alloc([128, 8, 512], BF16)
            Wv = ar.alloc([128, 8, 1024], BF16)
            Wo = ar.alloc([128, 8, 1024], BF16)
            Wgt = ar.alloc([128, 8, 8], BF16)
            Wout = ar.alloc([128, 8, 1024], BF16)
            bgb = ar.alloc([128, 8], F32)
            mng = ar.alloc([128, D], F32)
            hT2 = [ar.alloc([128, 8, 128], BF16) for _ in range(2)]
            hb2 = [hbf[:], ar.alloc([128, D], BF16)]
            Gtok = ar.alloc([128, 2, 64], F32)
            rows = [ar.alloc([64, 128], F32) for _ in range(5)]
            E1, L1, Bn, A_, U_ = rows
            sm = ar.alloc([64, 8], F32)
            rrow = ar.alloc([1, 3, 64], F32)
            MG = ar.alloc([64, 4], F32)
            Utok = ar.alloc([128, 64], F32)
            ENtok = ar.alloc([128, 64], F32)
            DEC = ar.alloc([128, 64], F32)
            qT2 = [ar.alloc([128, 4, 128], BF16) for _ in range(2)]
            kT2 = [ar.alloc([128, 4, 128], BF16) for _ in range(2)]
            ktok2 = [ar.alloc([128, 512], BF16) for _ in range(2)]
            vaug2 = [ar.alloc([128, 4, DVA], BF16) for _ in range(2)]
            uv2 = [ar.alloc([128, 4, DVA], BF16) for _ in range(2)]
            STb = ar.alloc([128, 4, 128], BF16)
            Gs2 = [ar.alloc([128, D], BF16) for _ in range(2)]
            C32 = ar.alloc([128, 4, DVA], F32)
            Cbf = ar.alloc([128, 4, DVA], BF16)
            ybf = ar.alloc([128, D], BF16)
            yT = ar.alloc([128, 8, 128], BF16)
            s4 = ar.alloc([128, 8, 4], F32)

            def wload(dst, src, key, res):
                dma("pool", dst, src.rearrange("(k p) n -> p k n", p=128), key, r=(), w=[res])
            wload(Wgt, mwin_d[j, :, 3072:3080], "w0", "Wgt")
            wload(Wq, mwin_d[j, :, 0:512], "w1", "Wq")
            wload(Wk, mwin_d[j, :, 512:1024], "w2", "Wk")
            wload(Wv, mwin_d[j, :, 1024:2048], "w3", "Wv")
            wload(Wo, mwin_d[j, :, 2048:3072], "w4", "Wo")
            wload(Wout, mwout_d[j], "w5", "Wout")
            dma("sp", bgb, mbg_d[j, :].partition_broadcast(128), "sm0", r=(), w=["bgb"])
            dma("sp", mng, mng_d[j, :].partition_broadcast(128), "sm1", r=(), w=["mng"])
            for p_ in range(2):
                memset("pool", vaug2[p_][:, :, 256:258], 1.0, [("vaug", p_)])

            def pre_norm(t):
                p_ = t % 2
                norm_tile(t, out_bf=hb2[p_], res_bf=("hb", p_))

            def pre_gates(t):
                p_ = t % 2
                bank = 6 + p_
                for k in range(8):
                    tr(psb[bank][:, k, :], hb2[p_][:, k * 128:(k + 1) * 128], ident_b[:], [("hb", p_)], [("ps", bank)])
                cp("act", hT2[p_], psb[bank], [("ps", bank)], [("hT", p_)])
                bg_ = 4 + p_
                for k in range(8):
                    mm(ps[bg_][:, 0:8], hT2[p_][:, k, :], Wgt[:, k, :], k == 0, k == 7,
                       r=[("hT", p_), "Wgt"], w=[("ps", bg_)])
                tt("dve", Gtok[:, :, t:64:16],
                   ps[bg_][:, 0:8].rearrange("p (g h) -> p g h", g=2), bgb[:].rearrange("p (g h) -> p g h", g=2),
                   ALU.add, [("ps", bg_), "bgb"], ["Gtok"])

            pre_norm(0)
            for t in range(NT):
                if t + 1 < NT:
                    pre_norm(t + 1)
                pre_gates(t)
            tr(ps[0][0:64, 0:128], Gtok[:, 0, :], ident_f[:], ["Gtok"], [("ps", 0)])
            tr(ps[1][0:64, 0:128], Gtok[:, 1, :], ident_f[:], ["Gtok"], [("ps", 1)])
            act(E1, ps[1][0:64, 0:128], AF.Exp, [("ps", 1)], ["E1"], scale=-1.0)
            act(L1, E1, AF.Ln, ["E1"], ["L1"], bias=1.0)
            P.add("dve", lambda e: e.tensor_tensor_scan(out=Bn, data0=L1, data1=L1, initial=0.0,
                                                        op0=ALU.add, op1=ALU.max), r=["L1"], w=["Bn"])
            mm(ps[2][0:64, 0:1], TT[:, :], Bn[:, 127:128], True, True, r=["TT", "Bn"], w=[("ps", 2)])
            ts("dve", Bn, Bn, ps[2][0:64, 0:1], ALU.add, ["Bn", ("ps", 2)], ["Bn"])
            tt("dve", A_, ps[0][0:64, 0:128], Bn, ALU.add, [("ps", 0), "Bn"], ["A"])
            P.add("dve", lambda e: e.tensor_reduce(out=sm[:, 0:1], in_=A_, axis=AX.X, op=ALU.max),
                  r=["A"], w=["rmax"])
            tr(ps[3][0:1, 0:64], sm[:, 0:1], ident_f[0:64, 0:64], ["rmax"], [("ps", 3)])
            cp("dve", rrow[:, 0, :], ps[3][0:1, 0:64], [("ps", 3)], ["rrow0"])
            for h in range(4):
                P.add("dve", lambda e, h=h: e.tensor_tensor_scan(
                    out=rrow[:, 1, h * 16:(h + 1) * 16], data0=rrow[:, 0, h * 16:(h + 1) * 16],
                    data1=rrow[:, 0, h * 16:(h + 1) * 16], initial=0.0, op0=ALU.max, op1=ALU.max),
                    r=["rrow0"], w=["rrow1"])
            memset("dve", rrow[:, 2, :], 0.0, ["rrow2"])
            cp("dve", rrow[:, 2, :].rearrange("p (h c) -> p h c", h=4)[:, :, 1:16],
               rrow[:, 1, :].rearrange("p (h c) -> p h c", h=4)[:, :, 0:15], ["rrow1", "rrow2"], ["rrow2"])
            tr(ps[4][0:64, 0:1], rrow[:, 2, :], ident_f[0:1, 0:1], ["rrow2"], [("ps", 4)])
            tr(ps[4][0:64, 1:2], rrow[:, 1, :], ident_f[0:1, 0:1], ["rrow1"], [("ps", 4)])
            cp("dve", MG[:, 0:2], ps[4][0:64, 0:2], [("ps", 4)], ["MG"])
            ts("dve", MG[:, 2:3], MG[:, 0:1], -1.0, ALU.mult, ["MG"], ["MGn"])
            tt("dve", MG[:, 3:4], MG[:, 0:1], MG[:, 1:2], ALU.subtract, ["MG"], ["MGd"])
            act(U_, A_, AF.Exp, ["A", "MGn"], ["U"], bias=MG[:, 2:3])
            act(E1, Bn, AF.Exp, ["Bn", "MGn"], ["EN"], bias=MG[:, 2:3])
            act(sm[:, 1:2], MG[:, 3:4], AF.Exp, ["MGd"], ["dec"])
            tr(ps[5][0:128, 0:64], U_, ident_f[0:64, 0:64], ["U"], [("ps", 5)])
            cp("dve", Utok, ps[5][:, 0:64], [("ps", 5)], ["Utok"])
            tr(ps[5][0:128, 64:128], E1, ident_f[0:64, 0:64], ["EN"], [("ps", 5)])
            cp("dve", ENtok, ps[5][:, 64:128], [("ps", 5)], ["ENtok"])
            tr(ps[3][0:1, 64:128], sm[:, 1:2], ident_f[0:64, 0:64], ["dec"], [("ps", 3)])
            cp("dve", rrow[:, 0, :], ps[3][0:1, 64:128], [("ps", 3)], ["rrow0"])
            mm(ps[2][:, 64:128], ones_f[0:1, :], rrow[:, 0, :], True, True, r=["ones_f", "rrow0"], w=[("ps", 2)])
            cp("dve", DEC, ps[2][:, 64:128], [("ps", 2)], ["DEC"])

            def stageA1(c):
                p_ = c % 2
                hT = hT2[p_]
                hTr = ("hT", p_)
                for k in range(8):
                    tr(psb[0][:, k, :], hb2[p_][:, k * 128:(k + 1) * 128], ident_b[:], [("hb", p_)], [("ps", 0)])
                cp("act", hT, psb[0], [("ps", 0)], [hTr])
                if c + 2 < NT:
                    norm_tile(c + 2, out_bf=hb2[p_], res_bf=("hb", p_))
                for h in range(4):
                    for k in range(8):
                        mm(ps[1][:, h * 128:(h + 1) * 128], Wq[:, k, h * 128:(h + 1) * 128], hT[:, k, :],
                           k == 0, k == 7, r=["Wq", hTr], w=[("ps", 1)])
                act(qT2[p_], ps[1].rearrange("p (h t) -> p h t", h=4), AF.Copy, [("ps", 1)], [("qT", p_)],
                    scale=float(128 ** -0.5))
                for h in range(4):
                    for k in range(8):
                        mm(ps[2][:, h * 128:(h + 1) * 128], Wk[:, k, h * 128:(h + 1) * 128], hT[:, k, :],
                           k == 0, k == 7, r=["Wk", hTr], w=[("ps", 2)])
                cp("dve", kT2[p_], ps[2].rearrange("p (h t) -> p h t", h=4), [("ps", 2)], [("kT", p_)])

            def stageA2(c):
                p_ = c % 2
                hT = hT2[p_]
                hTr = ("hT", p_)
                for k in range(8):
                    mm(ps[1], hT[:, k, :], Wk[:, k, :], k == 0, k == 7, r=["Wk", hTr], w=[("ps", 1)])
                cp("act", ktok2[p_], ps[1], [("ps", 1)], [("ktok", p_)])
                for hh in range(2):
                    b = 2 - hh
                    for k in range(8):
                        mm(ps[b], hT[:, k, :], Wv[:, k, hh * 512:(hh + 1) * 512], k == 0, k == 7,
                           r=["Wv", hTr], w=[("ps", b)])
                    cp("act", vaug2[p_][:, 2 * hh:2 * hh + 2, 0:256], ps[b].rearrange("p (h v) -> p h v", h=2),
                       [("ps", b)], [("vaug", p_)])
                for hh in range(2):
                    b = 2 - hh
                    for k in range(8):
                        mm(ps[b], hT[:, k, :], Wo[:, k, hh * 512:(hh + 1) * 512], k == 0, k == 7,
                           r=["Wo", hTr], w=[("ps", b)])
                    gsl = Gs2[p_][:, hh * 512:(hh + 1) * 512]
                    act(gsl, ps[b], AF.Sigmoid, [("ps", b)], [("Gs", p_, hh)])
                    tt("pool", gsl, gsl, mng[:, hh * 512:(hh + 1) * 512], ALU.mult, [("Gs", p_, hh), "mng"],
                       [("Gs", p_, hh)])
                tt("pool", uv2[p_][:, :, 0:NVA], vaug2[p_][:, :, 0:NVA],
                   Utok[:, c:64:16].unsqueeze(2).to_broadcast([128, 4, NVA]), ALU.mult,
                   [("vaug", p_), "Utok"], [("uv", p_)])

            def stageB1(c):
                p_ = c % 2
                for h in range(4):
                    mm(ps[3][:, h * 128:(h + 1) * 128], kT2[p_][:, h, :], qT2[p_][:, h, :], True, True,
                       r=[("kT", p_), ("qT", p_)], w=[("ps", 3)])
                tt("dve", STb, ps[3].rearrange("p (h t) -> p h t", h=4),
                   maskT[:].unsqueeze(1).to_broadcast([128, 4, 128]), ALU.mult, [("ps", 3), "maskT"], ["STb"])

            def stageB2(c):
                p_ = c % 2
                qT, ktok, uv = qT2[p_], ktok2[p_], uv2[p_]
                for h in range(4):
                    bn = 4 + h
                    mm(ps[bn][:, 0:NVA], STb[:, h, :], uv[:, h, 0:NVA], True, c == 0,
                       r=["STb", ("uv", p_)], w=[("ps", bn)])
                    if c > 0:
                        mm(ps[bn][:, 0:NVA], qT[:, h, :], Cbf[:, h, 0:NVA], False, True,
                           r=[("qT", p_), ("Cbf", h)], w=[("ps", bn)])
                if c < NT - 1:
                    for h in range(4):
                        bp = 1 + (h % 2)
                        col = h * 16 + c
                        mm(ps[bp][:, 0:NVA], ktok[:, h * 128:(h + 1) * 128], uv[:, h, 0:NVA], True, True,
                           r=[("ktok", p_), ("uv", p_)], w=[("ps", bp)])
                        if c == 0:
                            ts("dve", C32[:, h, 0:NVA], ps[bp][:, 0:NVA], DEC[:, col:col + 1], ALU.mult,
                               [("ps", bp), "DEC"], [("C32", h)])
                        else:
                            tt("dve", C32[:, h, 0:NVA], ps[bp][:, 0:NVA], C32[:, h, 0:NVA], ALU.add,
                               [("ps", bp), ("C32", h)], [("C32", h)])
                            act(C32[:, h, 0:NVA], C32[:, h, 0:NVA], AF.Copy, [("C32", h), "DEC"], [("C32", h)],
                                scale=DEC[:, col:col + 1])
                        cp("pool", Cbf[:, h, 0:NVA], C32[:, h, 0:NVA], [("C32", h)], [("Cbf", h)])

            def stageB3(c):
                p_ = c % 2
                Gs = Gs2[p_]
                nres = [("ps", 4 + h) for h in range(4)]
                den4 = psall[:, 4:8, 256]
                cp("dve", s4[:, 4, :], den4, nres, ["s4_4"])
                stt(s4[:, 7, :], s4[:, 4, :], -1.0, s4[:, 4, :], ALU.mult, ALU.max, ["s4_4"], ["s4_7"])
                tt("dve", s4[:, 0, :], s4[:, 7, :], ENtok[:, c:64:16], ALU.max, ["s4_7", "ENtok"], ["s4_0"])
                recip(s4[:, 1, :], s4[:, 0, :], ["s4_0"], ["s4_1"])
                for h in range(4):
                    act(junk[:, 0:256], ps[4 + h][:, 0:256], AF.Square, [("ps", 4 + h), "s4_1"],
                        ["junk", ("s4_2", h)], scale=s4[:, 1, h:h + 1], accum=s4[:, 2, h:h + 1])
                ts("dve", s4[:, 3, :], s4[:, 2, :], 1.0 / 256, ALU.mult, [("s4_2", h) for h in range(4)], ["s4_3"],
                   s2=EPS, op1=ALU.add)
                tt("pool", s4[:, 5, :], s4[:, 3, :], negh[:], ALU.pow, ["s4_3", "negh"], ["s4_5"])
                tt("dve", s4[:, 6, :], s4[:, 5, :], s4[:, 1, :], ALU.mult, ["s4_5", "s4_1"], ["s4_6"])
                for h in range(4):
                    stt(ybf[:, h * 256:(h + 1) * 256], ps[4 + h][:, 0:256], s4[:, 6, h:h + 1],
                        Gs[:, h * 256:(h + 1) * 256], ALU.mult, ALU.mult,
                        [("ps", 4 + h), "s4_6", ("Gs", p_, h // 2)], ["ybf"])

            def stageB4(c):
                for k in range(8):
                    tr(psb[0][:, k, :], ybf[:, k * 128:(k + 1) * 128], ident_b[:], ["ybf"], [("ps", 0)])
                cp("act", yT, psb[0], [("ps", 0)], ["yT"])
                for half in range(2):
                    b = 3 if half == 0 else 0
                    for k in range(8):
                        mm(ps[b], yT[:, k, :], Wout[:, k, half * 512:(half + 1) * 512], k == 0, k == 7,
                           r=["yT", "Wout"], w=[("ps", b)])
                    add_to_x(c, half, ps[b], ("ps", b))

            norm_tile(0, out_bf=hb2[0], res_bf=("hb", 0))
            norm_tile(1, out_bf=hb2[1], res_bf=("hb", 1))
            stageA1(0)
            stageA2(0)
            for c in range(NT):
                stageB1(c)
                if c + 1 < NT:
                    stageA1(c + 1)
                stageB2(c)
                stageB3(c)
                if c + 1 < NT:
                    stageA2(c + 1)
                stageB4(c)
            P.barrier()

        def sconv(j):
            ar.reset()
            Win = ar.alloc([128, 8, 3 * D], BF16)
            Wout = ar.alloc([128, 8, D], BF16)
            cw = ar.alloc([128, 8, 3], F32)
            hTf2 = [ar.alloc([128, 8, 512], BF16) for _ in range(2)]
            hb2 = [hbf[:], ar.alloc([128, D], BF16)]
            U = ar.alloc([128, 8, 516], F32)
            xbs2 = [ar.alloc([128, 512], F32) for _ in range(2)]
            Y2 = [ar.alloc([128, 512], F32) for _ in range(2)]
            zT = ar.alloc([128, 8, 512], BF16)
            for hf_ in range(2):
                for i3 in range(3):
                    c0 = i3 * D + hf_ * 512
                    dma("pool", Win[:, :, c0:c0 + 512],
                        swin_d[j, :, c0:c0 + 512].rearrange("(k p) n -> p k n", p=128), f"w{i3}",
                        r=(), w=[("Win", i3, hf_)])
            dma("pool", Wout, swout_d[j].rearrange("(k p) n -> p k n", p=128), "w3", r=(), w=["Wout"])
            dma("sp", cw, scw_d[j].rearrange("p (c t) -> p c t", t=3), "sm0", r=(), w=["cw"])
            memset("pool", U[:, :, 0:2], 0.0, [("U", f) for f in range(8)])

            def convA(n):
                p_ = n % 2
                for t4 in range(4):
                    t = n * 4 + t4
                    q_ = t4 % 2
                    norm_tile(t, out_bf=hb2[q_], res_bf=("hb", q_))
                    for k in range(8):
                        tr(psb[7][:, k, :], hb2[q_][:, k * 128:(k + 1) * 128], ident_b[:], [("hb", q_)], [("ps", 7)])
                    cp("act", hTf2[p_][:, :, t4 * 128:(t4 + 1) * 128], psb[7], [("ps", 7)], [("hTf", p_)])

            def convB(n):
                p_ = n % 2
                hTf = hTf2[p_]
                for f in range(8):
                    q_ = f % 2
                    bs = q_ * 3
                    xbs, Y = xbs2[q_], Y2[q_]
                    for i3 in (2, 1, 0):
                        for k in range(8):
                            mm(ps[bs + i3], Win[:, k, i3 * D + f * 128:i3 * D + (f + 1) * 128], hTf[:, k, :],
                               k == 0, k == 7, r=[("Win", i3, f // 4), ("hTf", p_)], w=[("ps", bs + i3)])
                    cp("act", xbs, ps[bs + 2], [("ps", bs + 2)], [("xbs", q_)])
                    tt("dve", U[:, f, 2:514], ps[bs + 1], xbs, ALU.mult, [("ps", bs + 1), ("xbs", q_)], [("U", f)])
                    ts("dve", Y, U[:, f, 2:514], cw[:, f, 2:3], ALU.mult, [("U", f), "cw"], [("Y", q_)])
                    stt(Y, U[:, f, 1:513], cw[:, f, 1:2], Y, ALU.mult, ALU.add, [("U", f), "cw", ("Y", q_)], [("Y", q_)])
                    stt(Y, U[:, f, 0:512], cw[:, f, 0:1], Y, ALU.mult, ALU.add, [("U", f), "cw", ("Y", q_)], [("Y", q_)])
                    tt("dve", zT[:, f, :], ps[bs], Y, ALU.mult, [("ps", bs), ("Y", q_)], ["zT"])
                    cp("pool", U[:, f, 0:2], U[:, f, 512:514], [("U", f)], [("U", f)])
                for t4 in range(4):
                    t = n * 4 + t4
                    for half in range(2):
                        b = 6 + half
                        for k in range(8):
                            mm(ps[b], zT[:, k, t4 * 128:(t4 + 1) * 128], Wout[:, k, half * 512:(half + 1) * 512],
                               k == 0, k == 7, r=["zT", "Wout"], w=[("ps", b)])
                        add_to_x(t, half, ps[b], ("ps", b))

            convA(0)
            for n in range(4):
                if n + 1 < 4:
                    convA(n + 1)
                convB(n)
            P.barrier()

        def moe(i, next_sub=None):
            ar.reset()
            DEST = ar.alloc([128, NT, 2], I32)
            WTS = ar.alloc([128, NT, 2], F32)
            mark = ar.off
            Wg = [ar.alloc([128, 8, 512], BF16) for _ in range(2)]
            Wu = [ar.alloc([128, 8, 512], BF16) for _ in range(2)]
            Wd = [ar.alloc([128, 4, D], BF16) for _ in range(2)]
            wend = ar.off
            ar.off = mark
            Wr = ar.alloc([128, 8, 36], F32)
            rb = ar.alloc([128, 36], F32)
            hfs = [ar.alloc([128, D], F32) for _ in range(2)]
            hTfs = [ar.alloc([128, 8, 128], F32) for _ in range(2)]
            ar_small_end = ar.off
            ar.off = max(ar.off, wend)
            HB = ar.alloc([128, NT, D], BF16)
            ar.off = ar_small_end
            LA = ar.alloc([128, NT, 36], F32)
            G4 = [ar.alloc([128, NT, 4], F32) for _ in range(4)]
            Lm = ar.alloc([128, NT, 32], F32)
            Lm2 = ar.alloc([128, NT, 32], F32)
            M1 = ar.alloc([128, NT, 32], F32)
            M2 = ar.alloc([128, NT, 32], F32)
            E = ar.alloc([128, NT, 32], F32)
            PRE = ar.alloc([128, NT, 32], F32)
            CNT = ar.alloc([128, NT, 32], F32)
            JK = ar.alloc([128, NT, 32], F32)
            SV = ar.alloc([128, 28, NT], F32)
            assert ar.off <= wend, ("routing scratch overruns weight slots", ar.off, wend)

            dma("sp", Wr, rw_d[i].rearrange("(k p) n -> p k n", p=128), "sm0", r=(), w=["Wr"])
            dma("sp", rb, rb_d[i, :].partition_broadcast(128), "sm1", r=(), w=["rb"])

            def load_expert(e):
                s_ = e % 2
                g = i * NE + e
                P.add("pool", lambda e, s_=s_, g=g: e.dma_start(
                    out=Wg[s_], in_=ewg_d[g].rearrange("(p k) n -> p k n", p=128), max_dma_last_dim=8192),
                    r=(), w=[("Wg", s_)], dma=f"w{s_}")
                P.add("pool", lambda e, s_=s_, g=g: e.dma_start(
                    out=Wu[s_], in_=ewu_d[g].rearrange("(p k) n -> p k n", p=128), max_dma_last_dim=8192),
                    r=(), w=[("Wu", s_)], dma=f"w{2 + s_}")
                dma("pool", Wd[s_], ewd_d[g].rearrange("(k p) n -> p k n", p=128), f"w{4 + s_}", r=(), w=[("Wd", s_)])

            def r1_norm(t):
                p_ = t % 2
                norm_tile(t, out_f=hfs[p_], res_f=("hf", p_))
                cp("pool", HB[:, t, :].rearrange("q (k p) -> q p k", p=128),
                   hfs[p_].rearrange("q (p k) -> q p k", k=8), [("hf", p_)], [("HB", t)])

            def r1_router(t):
                p_ = t % 2
                hf = hfs[p_]
                hTf = hTfs[p_]
                for g4 in range(2):
                    bk = p_ * 2 + g4
                    for k4 in range(4):
                        k = g4 * 4 + k4
                        tr(ps[bk][:, k4 * 128:(k4 + 1) * 128], hf[:, k * 128:(k + 1) * 128], ident_f[:],
                           [("hf", p_)], [("ps", bk)])
                    cp("act", hTf[:, g4 * 4:g4 * 4 + 4, :],
                       ps[bk].rearrange("p (k m) -> p k m", k=4), [("ps", bk)], [("hTf", p_, g4)])
                bl = 4 + p_
                for k in range(8):
                    mm(ps[bl][:, 0:36], hTf[:, k, :], Wr[:, k, :], k == 0, k == 7,
                       r=[("hTf", p_, k // 4), "Wr"], w=[("ps", bl)])
                tt("dve", LA[:, t, :], ps[bl][:, 0:36], rb, ALU.add, [("ps", bl), "rb"], ["LA"])

            r1_norm(0)
            for t in range(NT):
                if t + 1 < NT:
                    r1_norm(t + 1)
                r1_router(t)

            def bc(v, n):
                return v.unsqueeze(2).to_broadcast([128, NT, n])

            def red(out, in_, op, r, w):
                P.add("dve", lambda e: e.tensor_reduce(out=out, in_=in_, axis=AX.X, op=op), r=r, w=w)

            LA4 = LA[:, :, 0:4]
            LAE = LA[:, :, 4:36]
            red(SV[:, 0, :], LA4, ALU.max, ["LA"], ["gmax"])
            tt("dve", G4[0], LA4, bc(SV[:, 0, :], 4), ALU.subtract, ["LA", "gmax"], ["G40"])
            act(G4[1], G4[0], AF.Exp, ["G40"], ["G41"])
            red(SV[:, 1, :], G4[1], ALU.add, ["G41"], ["gsum"])
            recip(SV[:, 2, :], SV[:, 1, :], ["gsum"], ["psel"])
            tt("dve", G4[2], LA4, bc(SV[:, 0, :], 4), ALU.is_equal, ["LA", "gmax"], ["G42"])
            ts("dve", G4[3], G4[2], -1.0, ALU.add, ["G42"], ["G43"], s2=1.0e30, op1=ALU.mult)
            tt("dve", Lm.rearrange("p t (g e) -> p t g e", g=4), LAE.rearrange("p t (g e) -> p t g e", g=4),
               G4[3].unsqueeze(3).to_broadcast([128, NT, 4, 8]), ALU.add, ["LA", "G43"], ["Lm"])
            red(SV[:, 3, :], Lm, ALU.max, ["Lm"], ["m1"])
            tt("dve", M1, Lm, bc(SV[:, 3, :], 32), ALU.is_equal, ["Lm", "m1"], ["M1"])
            stt(Lm2, M1, -1.0e30, Lm, ALU.mult, ALU.add, ["M1", "Lm"], ["Lm2"])
            red(SV[:, 4, :], Lm2, ALU.max, ["Lm2"], ["m2"])
            tt("dve", M2, Lm2, bc(SV[:, 4, :], 32), ALU.is_equal, ["Lm2", "m2"], ["M2"])
            iob = iota_f[:].unsqueeze(1).to_broadcast([128, NT, 32])
            tt("dve", JK, M1, iob, ALU.mult, ["M1", "iota_f"], ["JK"])
            red(SV[:, 5, :], JK, ALU.add, ["JK"], ["eid0"])
            tt("dve", JK, M2, iob, ALU.mult, ["M2", "iota_f"], ["JK"])
            red(SV[:, 6, :], JK, ALU.add, ["JK"], ["eid1"])
            tt("dve", SV[:, 7, :], SV[:, 4, :], SV[:, 3, :], ALU.subtract, ["m1", "m2"], ["d"])
            act(SV[:, 8, :], SV[:, 7, :], AF.Exp, ["d"], ["e"])
            ts("dve", SV[:, 9, :], SV[:, 8, :], 1.0, ALU.add, ["e"], ["e1"])
            recip(SV[:, 10, :], SV[:, 9, :], ["e1"], ["r"])
            tt("dve", SV[:, 11, :], SV[:, 10, :], SV[:, 2, :], ALU.mult, ["r", "psel"], ["w0"])
            tt("dve", SV[:, 12, :], SV[:, 2, :], SV[:, 11, :], ALU.subtract, ["psel", "w0"], ["w1"])
            tt("dve", E, M1, M2, ALU.add, ["M1", "M2"], ["E"])
            Ef = E.rearrange("p t e -> p (t e)")
            mm(ps[6], Lt[:], Ef, True, True, r=["Lt", "E"], w=[("ps", 6)])
            mm(ps[7], ones_f[:], Ef, True, True, r=["ones_f", "E"], w=[("ps", 7)])
            memset("dve", PRE[:, 0, :], 0.0, ["PRE"])
            for t in range(1, NT):
                tt("dve", PRE[:, t, :], PRE[:, t - 1, :], ps[7][:, (t - 1) * 32:t * 32], ALU.add,
                   ["PRE", ("ps", 7)], ["PRE"])
            tt("dve", CNT.rearrange("p t e -> p (t e)"), PRE.rearrange("p t e -> p (t e)"), ps[6], ALU.add,
               ["PRE", ("ps", 6)], ["CNT"])
            for kk, MM in ((0, M1), (1, M2)):
                tt("dve", JK, MM, CNT, ALU.mult, ["M1", "M2", "CNT"], ["JK"])
                red(SV[:, 13 + kk, :], JK, ALU.add, ["JK"], [("pos", kk)])
                stt(SV[:, 15 + kk, :], SV[:, 5 + kk, :], float(CAP), SV[:, 13 + kk, :], ALU.mult, ALU.add,
                    ["eid0", "eid1", ("pos", kk)], [("dst", kk)])
                ts("dve", SV[:, 17 + kk, :], SV[:, 13 + kk, :], float(CAP), ALU.is_ge, [("pos", kk)], [("ov", kk)],
                   s2=1.0e6, op1=ALU.mult)
                tt("dve", SV[:, 15 + kk, :], SV[:, 15 + kk, :], SV[:, 17 + kk, :], ALU.add,
                   [("dst", kk), ("ov", kk)], [("dst", kk)])
                cp("dve", DEST[:, :, kk], SV[:, 15 + kk, :], [("dst", kk)], ["DEST"])
                ts("dve", SV[:, 19 + kk, :], SV[:, 13 + kk, :], float(CAP), ALU.is_lt, [("pos", kk)], [("keep", kk)])
                tt("dve", WTS[:, :, kk], SV[:, 11 + kk, :], SV[:, 19 + kk, :], ALU.mult,
                   ["w0", "w1", ("keep", kk)], ["WTS"])

            for t in range(NT):
                for kk in range(2):
                    P.add("pool", lambda e, t=t, kk=kk: e.indirect_dma_start(
                        out=Xd, out_offset=bass.IndirectOffsetOnAxis(ap=DEST[:, t, kk:kk + 1], axis=0),
                        in_=HB[:, t, :], in_offset=None, bounds_check=breg[0], oob_is_err=False),
                        r=["DEST", ("HB", t)], w=[("Xd", t, kk)], dma=f"sc{kk}")
            P.barrier()

            ar.off = wend
            Xtok = [ar.alloc([128, NJ, D], BF16) for _ in range(2)]
            XeT = [ar.alloc([128, 8, CAP], BF16) for _ in range(2)]
            sg = [ar.alloc([128, CAP], F32) for _ in range(2)]
            AT = [ar.alloc([128, 4, CAP], BF16) for _ in range(2)]
            yo_lo = ar.off
            Yo = [ar.alloc([128, D], BF16) for _ in range(4)]

            def x_load(e):
                s_ = e % 2
                dma("sp", Xtok[s_], Xd[e * CAP:(e + 1) * CAP, :].rearrange("(j p) d -> p j d", p=128), f"xl{s_}",
                    r=(), w=[("Xtok", s_)])

            def t_phase(e):
                s_ = e % 2
                for jj in range(NJ):
                    b = jj % 2
                    for k in range(8):
                        tr(psb[b][:, k, :], Xtok[s_][:, jj, k * 128:(k + 1) * 128], ident_b[:],
                           [("Xtok", s_)], [("ps", b)])
                    cp("act" if jj % 2 == 0 else "dve", XeT[s_][:, :, jj * 128:(jj + 1) * 128], psb[b],
                       [("ps", b)], [("XeT", s_, jj)])

            def gu_phase(e):
                s_ = e % 2
                xr = [("XeT", s_, jj) for jj in range(NJ)]
                for f in range(4):
                    bg = 2 + (f % 2) * 2
                    bu = bg + 1
                    for k in range(8):
                        mm(ps[bg], Wg[s_][:, k, f * 128:(f + 1) * 128], XeT[s_][:, k, :], k == 0, k == 7,
                           r=[("Wg", s_)] + xr, w=[("ps", bg)])
                    for k in range(8):
                        mm(ps[bu], Wu[s_][:, k, f * 128:(f + 1) * 128], XeT[s_][:, k, :], k == 0, k == 7,
                           r=[("Wu", s_)] + xr, w=[("ps", bu)])
                    act(sg[f % 2], ps[bg], AF.Silu, [("ps", bg)], [("sg", f % 2)])
                    tt("dve", AT[s_][:, f, :], ps[bu], sg[f % 2], ALU.mult, [("ps", bu), ("sg", f % 2)],
                       [("AT", s_, f)])

            yo_ctr = [0]

            def d_phase(e):
                s_ = e % 2
                ar_ = [("AT", s_, f) for f in range(4)]
                for jj in range(NJ):
                    yi = yo_ctr[0] % 4
                    yo_ctr[0] += 1
                    yo = Yo[yi]
                    yor = ("Yo", yi)
                    for half in range(2):
                        b = 6 + half
                        for f in range(4):
                            mm(ps[b], AT[s_][:, f, jj * 128:(jj + 1) * 128], Wd[s_][:, f, half * 512:(half + 1) * 512],
                               f == 0, f == 3, r=[("Wd", s_)] + ar_, w=[("ps", b)])
                        tt("dve", yo[:, half * 512:(half + 1) * 512], ps[b], modG[:, half * 512:(half + 1) * 512],
                           ALU.mult, [("ps", b), "modG"], [yor])
                    dma("sp", Yd[e * CAP + jj * 128:e * CAP + (jj + 1) * 128, :], yo, f"yo{yi}",
                        r=[yor], w=[("Yd", e, jj)])

            x_load(0)
            x_load(1)
            load_expert(0)
            t_phase(0)
            for e_ in range(NE):
                if e_ + 2 < NE:
                    x_load(e_ + 2)
                if e_ + 1 < NE:
                    load_expert(e_ + 1)
                gu_phase(e_)
                if e_ + 1 < NE:
                    t_phase(e_ + 1)
                d_phase(e_)
            P.barrier()

            nxt = None
            if next_sub is not None:
                yo_off = ar.off
                nxt = adaln_issue(next_sub, ng_d[next_sub, :], base=mark)
                assert ar.off <= yo_lo, ("adaLN prefetch overlaps live combine buffers", ar.off, yo_lo)
                ar.off = yo_off
            for yi in range(4):
                memset("pool", Yo[yi], 0.0, [("Yo", yi)])

            def gathers(t):
                for kk in range(2):
                    yi = (t % 2) * 2 + kk
                    P.add("pool", lambda e, t=t, kk=kk, yi=yi: e.indirect_dma_start(
                        out=Yo[yi][:], out_offset=None, in_=Yd,
                        in_offset=bass.IndirectOffsetOnAxis(ap=DEST[:, t, kk:kk + 1], axis=0),
                        bounds_check=breg[0], oob_is_err=False),
                        r=["DEST"], w=[("Yo", yi)], dma=f"ga{yi}")

            gathers(0)
            for t in range(NT):
                if t + 1 < NT:
                    gathers(t + 1)
                y0, y1 = (t % 2) * 2, (t % 2) * 2 + 1
                stt(xres[:, t, :], Yo[y0], WTS[:, t, 0:1], xres[:, t, :], ALU.mult, ALU.add,
                    [("Yo", y0), "WTS", ("x", t)], [("x", t)])
                stt(xres[:, t, :], Yo[y1], WTS[:, t, 1:2], xres[:, t, :], ALU.mult, ALU.add,
                    [("Yo", y1), "WTS", ("x", t)], [("x", t)])
            if nxt is not None:
                adaln_finish(nxt)
                return
            P.barrier()

        for t4 in range(4):
            dma("sp", xres[:, t4 * 4:(t4 + 1) * 4, :],
                x_d[t4 * 512:(t4 + 1) * 512, :].rearrange("(t p) d -> p t d", p=128),
                "xin", r=(), w=[("x", t) for t in range(t4 * 4, t4 * 4 + 4)])
        dma("sp", cs[:], c_d, "cin", r=(), w=["cs"])
        pre0 = [None]
        breg = [None]

        def _mk_breg(e):
            breg[0] = e.to_reg(NE * CAP - 1)
            return e.memset(ident_f[:], 0.0)
        P.add("pool", _mk_breg, r=(), w=["ident_f"])
        P.add("pool", lambda e: e.affine_select(out=ident_f[:], in_=ident_f[:], pattern=[[-1, 128]], base=0,
                                                channel_multiplier=1, compare_op=ALU.not_equal, fill=1.0),
              r=["ident_f"], w=["ident_f"])
        cp("pool", ident_b[:], ident_f[:], ["ident_f"], ["ident_b"])
        memset("pool", maskT[:], 1.0, ["maskT"])
        P.add("pool", lambda e: e.affine_select(out=maskT[:], in_=maskT[:], pattern=[[1, 128]], base=0,
                                                channel_multiplier=-1, compare_op=ALU.is_ge, fill=0.0),
              r=["maskT"], w=["maskT"])
        memset("pool", Lt[:], 1.0, ["Lt"])
        P.add("pool", lambda e: e.affine_select(out=Lt[:], in_=Lt[:], pattern=[[1, 128]], base=-1,
                                                channel_multiplier=-1, compare_op=ALU.is_ge, fill=0.0),
              r=["Lt"], w=["Lt"])
        memset("pool", ones_f[:], 1.0, ["ones_f"])
        memset("pool", TT[:], 1.0, ["TT"])
        P.add("pool", lambda e: e.affine_select(out=TT[:], in_=TT[:], pattern=[[1, 64]], base=-1,
                                                channel_multiplier=-1, compare_op=ALU.is_ge, fill=0.0),
              r=["TT"], w=["TT"])
        for hb in range(1, 4):
            P.add("pool", lambda e, hb=hb: e.affine_select(out=TT[:, 16 * hb:16 * hb + 16],
                                                           in_=TT[:, 16 * hb:16 * hb + 16],
                                                           pattern=[[0, 16]], base=-16 * hb, channel_multiplier=1,
                                                           compare_op=ALU.is_ge, fill=0.0),
                  r=["TT"], w=["TT"])
        P.add("pool", lambda e: e.iota(iota_i[:], pattern=[[1, 32]], base=0, channel_multiplier=0),
              r=(), w=["iota_i"])
        cp("pool", iota_f[:], iota_i[:], ["iota_i"], ["iota_f"])
        act(sc[:], cs[:], AF.Silu, ["cs"], ["sc"])
        memset("pool", negh[:], -0.5, ["negh"])
        pre0[0] = adaln_issue(0, ng_d[0, :])
        P.barrier()


        pre_done = False
        for sub in range(n_sub):
            i, s_ = sub // 2, sub % 2
            if not pre_done:
                if sub == 0:
                    adaln_finish(pre0[0])
                else:
                    adaln(sub, ng_d[sub, :])
            pre_done = False
            if s_ == 0:
                if i % 2 == 0:
                    mlstm(i // 2)
                else:
                    sconv(i // 2)
            else:
                if sub + 1 < n_sub:
                    moe(i, next_sub=sub + 1)
                    pre_done = True
                else:
                    moe(i)

        ar.reset()
        if do_final:
            dma("sp", modA[:], fg_d[0, :].partition_broadcast(128), "gbc", r=(), w=["modA"])
            compute_rstd()
        outs = [ar.alloc([128, D], F32) for _ in range(2)]
        for t in range(NT):
            o = outs[t % 2]
            if do_final:
                norm_tile(t, out_f=o, shift=False)
                dma("sp", y_d[t * 128:(t + 1) * 128, :], o, f"out{t % 2}", r=["hf"], w=[("y", t)])
            else:
                dma("sp", y_d[t * 128:(t + 1) * 128, :], xres[:, t, :], f"out{t % 2}", r=[("x", t)], w=[("y", t)])

        dma_keys = sorted(P.dma_cnt.keys())
        eng_sems = {e: es.enter_context(nc.semaphore(f"s_{e}")) for e in ENGS}
        dma_sems = {k: es.enter_context(nc.semaphore(f"d_{k}")) for k in dma_keys}
        bodies = P.emit(None, eng_sems, dma_sems)
        with nc.Block() as block:
            block.tensor(bodies["pe"])
            block.scalar(bodies["act"])
            block.vector(bodies["dve"])
            block.gpsimd(bodies["pool"])
            block.sync(bodies["sp"])
        n_ops = {e: len(P.ops[e]) for e in ENGS}
    return nc, n_ops


def make_in_maps(inputs):
    f = lambda a: np.ascontiguousarray(np.asarray(a, dtype=np.float32))
    x = f(inputs["x"])
    c = f(inputs["c"])
    B = x.shape[0]
    shared = {
        "ada_w": f(inputs["ada_w"]).reshape(2 * DEPTH, D, 3 * D),
        "ada_b": f(inputs["ada_b"]).reshape(2 * DEPTH, 3 * D),
        "norm_g": f(inputs["norm_g"]).reshape(2 * DEPTH, D),
        "final_g": f(inputs["final_g"]).reshape(1, D),
        "m_w_in": f(inputs["m_w_in"]),
        "m_b_gates": f(inputs["m_b_gates"]),
        "m_norm_g": f(inputs["m_norm_g"]),
        "m_w_out": f(inputs["m_w_out"]),
        "s_w_in": f(inputs["s_w_in"]),
        "s_conv_w": np.ascontiguousarray(
            f(inputs["s_conv_w"]).reshape(2, 3, 8, 128).transpose(0, 3, 2, 1).reshape(2, 128, 24)),
        "s_w_out": f(inputs["s_w_out"]),
        "r_w": np.ascontiguousarray(np.concatenate([f(inputs["r_w_group"]), f(inputs["r_w_expert"])], axis=-1)),
        "r_b": np.ascontiguousarray(np.concatenate([f(inputs["r_b_group"]), f(inputs["r_b_expert"])], axis=-1)),
        "e_w_gate": f(inputs["e_w_gate"]).reshape(DEPTH * NE, D, 512),
        "e_w_up": f(inputs["e_w_up"]).reshape(DEPTH * NE, D, 512),
        "e_w_down": f(inputs["e_w_down"]).reshape(DEPTH * NE, 512, D),
    }
    maps = []
    for b in range(B):
        m = dict(shared)
        m["x"] = np.ascontiguousarray(x[b])
        m["c"] = np.ascontiguousarray(c[b].reshape(8, 128).T)
        maps.append(m)
    return maps


def kernel(**inputs):
    nc, _ = build_program()
    in_maps = make_in_maps(inputs)
    res = run_bass_kernel_spmd(nc, in_maps, core_ids=list(range(len(in_maps))))
    return np.stack([r["y"] for r in res.results], axis=0).astype(np.float32)
```

```python
import numpy as np
import concourse.bass as bass
import concourse.mybir as mybir
from concourse.bass_utils import run_bass_kernel_spmd

F32 = mybir.dt.float32
BF16 = mybir.dt.bfloat16
I32 = mybir.dt.int32
U32 = mybir.dt.uint32
AF = mybir.ActivationFunctionType
ALU = mybir.AluOpType
AX = mybir.AxisListType

S = 2048
D = 1024
NT = 16
DEPTH = 4
NH = 4
DVA = 258
NVA = 257
NE = 32
CAP = 512
NJ = CAP // 128
EPS = 1e-6
ENGS = ("pe", "act", "dve", "pool", "sp")


class Prog:
    def __init__(self, nc):
        self.nc = nc
        self.ops = {e: [] for e in ENGS}
        self.last_w = {}
        self.readers = {}
        self.dma_cnt = {}
        self.pending = {e: {} for e in ENGS}
        self.last_compute = {e: None for e in ENGS}
        self.bg_keys = set()

    @staticmethod
    def _merge(dst, ev, kind):
        k = ev[:2]
        old = dst.get(k)
        if old is None:
            old = [None, None]
            dst[k] = old
        if old[0] is None or old[0][2] < ev[2]:
            old[0] = ev
        if kind != "war" and (old[1] is None or old[1][2] < ev[2]):
            old[1] = ev

    def add(self, eng, fn, r=(), w=(), dma=None):
        idx = len(self.ops[eng])
        deps = {}
        for k, v in self.pending[eng].items():
            deps[k] = list(v)
        self.pending[eng] = {}
        for res in r:
            ev = self.last_w.get(res)
            if ev is not None:
                self._merge(deps, ev, "raw")
        for res in w:
            ev = self.last_w.get(res)
            if ev is not None:
                self._merge(deps, ev, "waw")
            for ev2 in self.readers.get(res, {}).values():
                self._merge(deps, ev2, "war")
        if dma is not None:
            self.dma_cnt[dma] = self.dma_cnt.get(dma, 0) + 16
            myev = ("d", dma, self.dma_cnt[dma])
        else:
            myev = ("c", eng, idx)
            self.last_compute[eng] = myev
        need = []
        for ev_any, ev_nw in deps.values():
            if ev_any[0] == "c" and ev_any[1] == eng and dma is None and eng == "pe":
                continue
            need.append(ev_any)
        self.ops[eng].append(dict(fn=fn, deps=need, ev=myev, dma=dma))
        for res in r:
            self.readers.setdefault(res, {})[myev[:2]] = myev
        for res in w:
            self.last_w[res] = myev
            self.readers[res] = {}
        return myev

    def barrier(self):
        evs = []
        for e in ENGS:
            if self.last_compute[e] is not None:
                evs.append(self.last_compute[e])
        for k, c in self.dma_cnt.items():
            if k not in self.bg_keys:
                evs.append(("d", k, c))
        for e in ENGS:
            for ev in evs:
                self._merge(self.pending[e], ev, "raw")
        self.last_w = {}
        self.readers = {}

    def emit(self, handles, eng_sems, dma_sems):
        sig = {e: set() for e in ENGS}
        for e in ENGS:
            for op in self.ops[e]:
                for ev in op["deps"]:
                    if ev[0] == "c":
                        sig[ev[1]].add(ev[2])
        rank = {}
        for e in ENGS:
            n = 0
            rank[e] = {}
            for i in range(len(self.ops[e])):
                if i in sig[e]:
                    n += 1
                    rank[e][i] = n

        def run(eng):
            def body(h):
                waited = {}
                for i, op in enumerate(self.ops[eng]):
                    for ev in op["deps"]:
                        if ev[0] == "c":
                            key = ("c", ev[1])
                            val = rank[ev[1]][ev[2]]
                            sem = eng_sems[ev[1]]
                        else:
                            key = ("d", ev[1])
                            val = ev[2]
                            sem = dma_sems[ev[1]]
                        if waited.get(key, 0) >= val:
                            continue
                        h.wait_ge(sem, val)
                        waited[key] = val
                    inst = op["fn"](h)
                    if op["dma"] is not None:
                        inst.then_inc(dma_sems[op["dma"]], 16)
                    elif i in sig[eng]:
                        inst.then_inc(eng_sems[eng], 1)
                if eng == "sp":
                    for k, c in self.dma_cnt.items():
                        if waited.get(("d", k), 0) < c:
                            h.wait_ge(dma_sems[k], c)
            return body
        return {e: run(e) for e in ENGS}


class Arena:
    def __init__(self, t, nbytes):
        self.t = t
        self.n = nbytes
        self.off = 0

    def reset(self):
        self.off = 0

    def alloc(self, shape, dt):
        esz = 4 if dt in (F32, I32, U32) else 2
        per = 1
        for s in shape[1:]:
            per *= s
        nb = (per * esz + 63) // 64 * 64
        assert self.off + nb <= self.n - 1024, ("arena overflow", self.off, nb, self.n)
        v = self.t[:, self.off // 4:(self.off + nb) // 4]
        self.off += nb
        if dt != F32:
            v = v.bitcast(dt)
        v = v[0:shape[0], 0:per]
        if len(shape) == 3:
            v = v.rearrange("p (a b) -> p a b", a=shape[1])
        elif len(shape) == 4:
            v = v.rearrange("p (a b c) -> p a b c", a=shape[1], b=shape[2])
        return v


def build_program(n_sub=2 * DEPTH, do_final=True):
    nc = bass.Bass("TRN2", target_bir_lowering=False)

    def din(name, shape, dt=F32):
        return nc.dram_tensor(name, shape, dt, kind="ExternalInput").ap()

    x_d = din("x", [S, D])
    c_d = din("c", [128, 8])
    adaw_d = din("ada_w", [2 * DEPTH, D, 3 * D])
    adab_d = din("ada_b", [2 * DEPTH, 3 * D])
    ng_d = din("norm_g", [2 * DEPTH, D])
    fg_d = din("final_g", [1, D])
    mwin_d = din("m_w_in", [2, D, 3080])
    mbg_d = din("m_b_gates", [2, 8])
    mng_d = din("m_norm_g", [2, D])
    mwout_d = din("m_w_out", [2, D, D])
    swin_d = din("s_w_in", [2, D, 3 * D])
    scw_d = din("s_conv_w", [2, 128, 24])
    swout_d = din("s_w_out", [2, D, D])
    rw_d = din("r_w", [DEPTH, D, 36])
    rb_d = din("r_b", [DEPTH, 36])
    ewg_d = din("e_w_gate", [DEPTH * NE, D, 512])
    ewu_d = din("e_w_up", [DEPTH * NE, D, 512])
    ewd_d = din("e_w_down", [DEPTH * NE, 512, D])
    y_d = nc.dram_tensor("y", [S, D], F32, kind="ExternalOutput").ap()
    Xd = nc.dram_tensor("xd_scr", [NE * CAP, D], BF16, kind="Internal").ap()
    Yd = nc.dram_tensor("yd_scr", [NE * CAP, D], BF16, kind="Internal").ap()

    ARENA_BYTES = 116 * 1024
    from contextlib import ExitStack
    with ExitStack() as es:
        def sb(name, shape, dt=F32):
            return es.enter_context(nc.sbuf_tensor(name, shape, dt))

        xres = sb("xres", [128, NT, D])
        modA = sb("modA", [128, D])
        modB = sb("modB", [128, D])
        modG = sb("modG", [128, D])
        ident_f = sb("ident_f", [128, 128])
        ident_b = sb("ident_b", [128, 128], BF16)
        maskT = sb("maskT", [128, 128])
        Lt = sb("Lt", [128, 128])
        ones_f = sb("ones_f", [128, 128])
        TT = sb("TT", [64, 64])
        iota_i = sb("iota_i", [128, 32], I32)
        iota_f = sb("iota_f", [128, 32])
        cs = sb("cs", [128, 8])
        sc = sb("sc", [128, 8])
        junk = sb("junk", [128, D], BF16)
        tmpf = sb("tmpf", [128, D])
        hbf = sb("hbf", [128, D], BF16)
        st = sb("st", [128, 16])
        RSTD = sb("RSTD", [128, NT])
        SSt = sb("SSt", [128, NT])
        ntmp = sb("ntmp", [128, D])
        arena_t = sb("arena", [128, ARENA_BYTES // 4])
        ar = Arena(arena_t, ARENA_BYTES)
        ztile = arena_t[:, (ARENA_BYTES - 1024) // 4:ARENA_BYTES // 4].bitcast(BF16)
        xdz_ev = [None]
        psall = es.enter_context(nc.psum_tensor("psall", [128, 8, 512], F32))
        ps = [psall[:, i, :] for i in range(8)]
        psb = [psall[:, i, :].bitcast(BF16).rearrange("p (k m) -> p k m", k=8) for i in range(8)]
        negh = sb("negh", [128, 4])

        P = Prog(nc)

        def mm(out, lhsT, rhs, start, stop, r, w):
            P.add("pe", lambda e: e.matmul(out, lhsT=lhsT, rhs=rhs, start=start, stop=stop), r=r, w=w)

        def tr(out, in_, ident, r, w):
            P.add("pe", lambda e: e.transpose(out=out, in_=in_, identity=ident), r=r, w=w)

        def act(out, in_, func, r, w, scale=1.0, bias=0.0, accum=None, eng="act"):
            P.add(eng, lambda e: e.activation(out=out, in_=in_, func=func, bias=bias, scale=scale,
                                              accum_out=accum), r=r, w=w)

        def tt(eng, out, in0, in1, op, r, w):
            P.add(eng, lambda e: e.tensor_tensor(out=out, in0=in0, in1=in1, op=op), r=r, w=w)

        def ts(eng, out, in0, s1, op0, r, w, s2=None, op1=None):
            if op1 is None:
                P.add(eng, lambda e: e.tensor_scalar(out=out, in0=in0, scalar1=s1, scalar2=None, op0=op0), r=r, w=w)
            else:
                P.add(eng, lambda e: e.tensor_scalar(out=out, in0=in0, scalar1=s1, scalar2=s2, op0=op0, op1=op1),
                      r=r, w=w)

        def stt(out, in0, scalar, in1, op0, op1, r, w):
            P.add("dve", lambda e: e.scalar_tensor_tensor(out=out, in0=in0, scalar=scalar, in1=in1,
                                                          op0=op0, op1=op1), r=r, w=w)

        def cp(eng, out, in_, r, w):
            if eng == "act":
                P.add("act", lambda e: e.copy(out=out, in_=in_), r=r, w=w)
            else:
                P.add(eng, lambda e: e.tensor_copy(out=out, in_=in_), r=r, w=w)

        def recip(out, in_, r, w):
            P.add("dve", lambda e: e.reciprocal(out=out, in_=in_), r=r, w=w)

        def dma(q, out, in_, key, r, w):
            P.add(q, lambda e: e.dma_start(out=out, in_=in_), r=r, w=w, dma=key)

        def memset(eng, ap, val, w):
            P.add(eng, lambda e: e.memset(ap, val), r=(), w=w)

        def compute_rstd():
            for t in range(NT):
                act(junk[:], xres[:, t, :], AF.Square, [("x", t)], ["junk", "SSt"], accum=SSt[:, t:t + 1])
            ts("dve", SSt[:], SSt[:], 1.0 / D, ALU.mult, ["SSt"], ["SSt"], s2=EPS, op1=ALU.add)
            act(SSt[:], SSt[:], AF.Sqrt, ["SSt"], ["SSt"])
            recip(RSTD[:], SSt[:], ["SSt"], ["RSTD"])

        def adaln_issue(sub, g_src, base=0):
            ar.off = base
            bufs = dict(
                SCB=ar.alloc([128, 8, 128], BF16),
                WA=[ar.alloc([128, 8, D], BF16) for _ in range(3)],
                adab=ar.alloc([128, 3 * D], F32),
                gbc=ar.alloc([128, D], F32),
            )
            for w3 in range(3):
                dma("pool", bufs["WA"][w3],
                    adaw_d[sub, :, w3 * D:(w3 + 1) * D].rearrange("(k p) n -> p k n", p=128),
                    f"wa{w3}", r=(), w=[("WA", w3)])
            dma("sp", bufs["adab"], adab_d[sub, :].partition_broadcast(128), "adab", r=(), w=["adab"])
            dma("sp", bufs["gbc"], g_src.partition_broadcast(128), "gbc", r=(), w=["gbc"])
            return bufs

        def adaln_finish(bufs):
            SCB, WA, adab, gbc = bufs["SCB"], bufs["WA"], bufs["adab"], bufs["gbc"]
            cp("dve", SCB, sc[:].unsqueeze(2).to_broadcast([128, 8, 128]), ["sc"], ["SCB"])
            compute_rstd()
            for blk in range(6):
                b = blk % 2
                which = blk // 2
                hcol = (blk % 2) * 512
                for k in range(8):
                    mm(ps[b], SCB[:, k, :], WA[which][:, k, hcol:hcol + 512], k == 0, k == 7,
                       r=["SCB", ("WA", which)], w=[("ps", b)])
                half = slice(hcol, hcol + 512)
                cols = slice(blk * 512, blk * 512 + 512)
                if which == 0:
                    tt("dve", modB[:, half], ps[b], adab[:, cols], ALU.add, [("ps", b), "adab"], ["modB"])
                elif which == 1:
                    tt("dve", tmpf[:, half], ps[b], adab[:, cols], ALU.add, [("ps", b), "adab"], ["tmpf"])
                    stt(modA[:, half], tmpf[:, half], 1.0, gbc[:, half], ALU.add, ALU.mult,
                        ["tmpf", "gbc"], ["modA"])
                else:
                    tt("dve", modG[:, half], ps[b], adab[:, cols], ALU.add, [("ps", b), "adab"], ["modG"])
            P.barrier()

        def adaln(sub, g_src):
            adaln_finish(adaln_issue(sub, g_src))

        def norm_tile(t, out_bf=None, out_f=None, shift=True, res_f="hf", res_bf="hbf"):
            xr = ("x", t)
            if shift:
                stt(ntmp[:], xres[:, t, :], RSTD[:, t:t + 1], modA[:], ALU.mult, ALU.mult,
                    [xr, "RSTD", "modA"], ["ntmp"])
                if out_f is not None:
                    tt("dve", out_f, ntmp[:], modB[:], ALU.add, ["ntmp", "modB"], [res_f])
                    if out_bf is not None:
                        cp("act", out_bf, out_f, [res_f], [res_bf])
                else:
                    tt("dve", out_bf, ntmp[:], modB[:], ALU.add, ["ntmp", "modB"], [res_bf])
            else:
                stt(out_f, xres[:, t, :], RSTD[:, t:t + 1], modA[:], ALU.mult, ALU.mult,
                    [xr, "RSTD", "modA"], [res_f])

        def transpose_tile_bf(src, src_res, dst, dst_res, bank):
            for k in range(8):
                tr(psb[bank][:, k, :], src[:, k * 128:(k + 1) * 128], ident_b[:], [src_res], [("ps", bank)])
            cp("act", dst, psb[bank], [("ps", bank)], [dst_res])

        def add_to_x(t, half, psum_ap, psres):
            hs = slice(half * 512, half * 512 + 512)
            tt("dve", tmpf[:, hs], psum_ap, modG[:, hs], ALU.mult, [psres, "modG"], ["tmpf"])
            tt("pool", xres[:, t, hs], xres[:, t, hs], tmpf[:, hs], ALU.add, [("x", t), "tmpf"], [("x", t)])

        def mlstm(j):
            ar.reset()
            Wq = ar.alloc([128, 8, 512], BF16)
            Wk = ar.alloc([128, 8, 512], BF16)
            Wv = ar.alloc([128, 8, 1024], BF16)
            Wo = ar.alloc([128, 8, 1024], BF16)
            Wgt = ar.alloc([128, 8, 8], BF16)
            Wout = ar.alloc([128, 8, 1024], BF16)
            bgb = ar.alloc([128, 8], F32)
            mng = ar.alloc([128, D], F32)
            hT2 = [ar.alloc([128, 8, 128], BF16) for _ in range(2)]
            hb2 = [hbf[:], ar.alloc([128, D], BF16)]
            Gtok = ar.alloc([128, 2, 64], F32)
            rows = [ar.alloc([64, 128], F32) for _ in range(5)]
            E1, L1, Bn, A_, U_ = rows
            sm = ar.alloc([64, 8], F32)
            rrow = ar.alloc([1, 3, 64], F32)
            MG = ar.alloc([64, 4], F32)
            Utok = ar.alloc([128, 64], F32)
            ENtok = ar.alloc([128, 64], F32)
            DEC = ar.alloc([128, 64], F32)
            qT2 = [ar.alloc([128, 4, 128], BF16) for _ in range(2)]
            kT2 = [ar.alloc([128, 4, 128], BF16) for _ in range(2)]
            ktok2 = [ar.alloc([128, 512], BF16) for _ in range(2)]
            vaug2 = [ar.alloc([128, 4, DVA], BF16) for _ in range(2)]
            uv2 = [ar.alloc([128, 4, DVA], BF16) for _ in range(2)]
            STb = ar.alloc([128, 4, 128], BF16)
            Gs2 = [ar.alloc([128, D], BF16) for _ in range(2)]
            C32 = ar.alloc([128, 4, DVA], F32)
            Cbf = ar.alloc([128, 4, DVA], BF16)
            ybf = ar.alloc([128, D], BF16)
            yT = ar.alloc([128, 8, 128], BF16)
            s4 = ar.alloc([128, 8, 4], F32)

            def wload(dst, src, key, res):
                dma("pool", dst, src.rearrange("(k p) n -> p k n", p=128), key, r=(), w=[res])
            wload(Wgt, mwin_d[j, :, 3072:3080], "w0", "Wgt")
            wload(Wq, mwin_d[j, :, 0:512], "w1", "Wq")
            wload(Wk, mwin_d[j, :, 512:1024], "w2", "Wk")
            wload(Wv, mwin_d[j, :, 1024:2048], "w3", "Wv")
            wload(Wo, mwin_d[j, :, 2048:3072], "w4", "Wo")
            wload(Wout, mwout_d[j], "w5", "Wout")
            dma("sp", bgb, mbg_d[j, :].partition_broadcast(128), "sm0", r=(), w=["bgb"])
            dma("sp", mng, mng_d[j, :].partition_broadcast(128), "sm1", r=(), w=["mng"])
            for p_ in range(2):
                memset("pool", vaug2[p_][:, :, 256:258], 1.0, [("vaug", p_)])

            def pre_norm(t):
                p_ = t % 2
                norm_tile(t, out_bf=hb2[p_], res_bf=("hb", p_))

            def pre_gates(t):
                p_ = t % 2
                bank = 6 + p_
                for k in range(8):
                    tr(psb[bank][:, k, :], hb2[p_][:, k * 128:(k + 1) * 128], ident_b[:], [("hb", p_)], [("ps", bank)])
                cp("act", hT2[p_], psb[bank], [("ps", bank)], [("hT", p_)])
                bg_ = 4 + p_
                for k in range(8):
                    mm(ps[bg_][:, 0:8], hT2[p_][:, k, :], Wgt[:, k, :], k == 0, k == 7,
                       r=[("hT", p_), "Wgt"], w=[("ps", bg_)])
                tt("dve", Gtok[:, :, t:64:16],
                   ps[bg_][:, 0:8].rearrange("p (g h) -> p g h", g=2), bgb[:].rearrange("p (g h) -> p g h", g=2),
                   ALU.add, [("ps", bg_), "bgb"], ["Gtok"])

            pre_norm(0)
            for t in range(NT):
                if t + 1 < NT:
                    pre_norm(t + 1)
                pre_gates(t)
            tr(ps[0][0:64, 0:128], Gtok[:, 0, :], ident_f[:], ["Gtok"], [("ps", 0)])
            tr(ps[1][0:64, 0:128], Gtok[:, 1, :], ident_f[:], ["Gtok"], [("ps", 1)])
            act(E1, ps[1][0:64, 0:128], AF.Exp, [("ps", 1)], ["E1"], scale=-1.0)
            act(L1, E1, AF.Ln, ["E1"], ["L1"], bias=1.0)
            P.add("dve", lambda e: e.tensor_tensor_scan(out=Bn, data0=L1, data1=L1, initial=0.0,
                                                        op0=ALU.add, op1=ALU.max), r=["L1"], w=["Bn"])
            mm(ps[2][0:64, 0:1], TT[:, :], Bn[:, 127:128], True, True, r=["TT", "Bn"], w=[("ps", 2)])
            ts("dve", Bn, Bn, ps[2][0:64, 0:1], ALU.add, ["Bn", ("ps", 2)], ["Bn"])
            tt("dve", A_, ps[0][0:64, 0:128], Bn, ALU.add, [("ps", 0), "Bn"], ["A"])
            P.add("dve", lambda e: e.tensor_reduce(out=sm[:, 0:1], in_=A_, axis=AX.X, op=ALU.max),
                  r=["A"], w=["rmax"])
            tr(ps[3][0:1, 0:64], sm[:, 0:1], ident_f[0:64, 0:64], ["rmax"], [("ps", 3)])
            cp("dve", rrow[:, 0, :], ps[3][0:1, 0:64], [("ps", 3)], ["rrow0"])
            for h in range(4):
                P.add("dve", lambda e, h=h: e.tensor_tensor_scan(
                    out=rrow[:, 1, h * 16:(h + 1) * 16], data0=rrow[:, 0, h * 16:(h + 1) * 16],
                    data1=rrow[:, 0, h * 16:(h + 1) * 16], initial=0.0, op0=ALU.max, op1=ALU.max),
                    r=["rrow0"], w=["rrow1"])
            memset("dve", rrow[:, 2, :], 0.0, ["rrow2"])
            cp("dve", rrow[:, 2, :].rearrange("p (h c) -> p h c", h=4)[:, :, 1:16],
               rrow[:, 1, :].rearrange("p (h c) -> p h c", h=4)[:, :, 0:15], ["rrow1", "rrow2"], ["rrow2"])
            tr(ps[4][0:64, 0:1], rrow[:, 2, :], ident_f[0:1, 0:1], ["rrow2"], [("ps", 4)])
            tr(ps[4][0:64, 1:2], rrow[:, 1, :], ident_f[0:1, 0:1], ["rrow1"], [("ps", 4)])
            cp("dve", MG[:, 0:2], ps[4][0:64, 0:2], [("ps", 4)], ["MG"])
            ts("dve", MG[:, 2:3], MG[:, 0:1], -1.0, ALU.mult, ["MG"], ["MGn"])
            tt("dve", MG[:, 3:4], MG[:, 0:1], MG[:, 1:2], ALU.subtract, ["MG"], ["MGd"])
            act(U_, A_, AF.Exp, ["A", "MGn"], ["U"], bias=MG[:, 2:3])
            act(E1, Bn, AF.Exp, ["Bn", "MGn"], ["EN"], bias=MG[:, 2:3])
            act(sm[:, 1:2], MG[:, 3:4], AF.Exp, ["MGd"], ["dec"])
            tr(ps[5][0:128, 0:64], U_, ident_f[0:64, 0:64], ["U"], [("ps", 5)])
            cp("dve", Utok, ps[5][:, 0:64], [("ps", 5)], ["Utok"])
            tr(ps[5][0:128, 64:128], E1, ident_f[0:64, 0:64], ["EN"], [("ps", 5)])
            cp("dve", ENtok, ps[5][:, 64:128], [("ps", 5)], ["ENtok"])
            tr(ps[3][0:1, 64:128], sm[:, 1:2], ident_f[0:64, 0:64], ["dec"], [("ps", 3)])
            cp("dve", rrow[:, 0, :], ps[3][0:1, 64:128], [("ps", 3)], ["rrow0"])
            mm(ps[2][:, 64:128], ones_f[0:1, :], rrow[:, 0, :], True, True, r=["ones_f", "rrow0"], w=[("ps", 2)])
            cp("dve", DEC, ps[2][:, 64:128], [("ps", 2)], ["DEC"])

            def stageA1(c):
                p_ = c % 2
                hT = hT2[p_]
                hTr = ("hT", p_)
                for k in range(8):
                    tr(psb[0][:, k, :], hb2[p_][:, k * 128:(k + 1) * 128], ident_b[:], [("hb", p_)], [("ps", 0)])
                cp("act", hT, psb[0], [("ps", 0)], [hTr])
                if c + 2 < NT:
                    norm_tile(c + 2, out_bf=hb2[p_], res_bf=("hb", p_))
                for h in range(4):
                    for k in range(8):
                        mm(ps[1][:, h * 128:(h + 1) * 128], Wq[:, k, h * 128:(h + 1) * 128], hT[:, k, :],
                           k == 0, k == 7, r=["Wq", hTr], w=[("ps", 1)])
                act(qT2[p_], ps[1].rearrange("p (h t) -> p h t", h=4), AF.Copy, [("ps", 1)], [("qT", p_)],
                    scale=float(128 ** -0.5))
                for h in range(4):
                    for k in range(8):
                        mm(ps[2][:, h * 128:(h + 1) * 128], Wk[:, k, h * 128:(h + 1) * 128], hT[:, k, :],
                           k == 0, k == 7, r=["Wk", hTr], w=[("ps", 2)])
                cp("dve", kT2[p_], ps[2].rearrange("p (h t) -> p h t", h=4), [("ps", 2)], [("kT", p_)])

            def stageA2(c):
                p_ = c % 2
                hT = hT2[p_]
                hTr = ("hT", p_)
                for k in range(8):
                    mm(ps[1], hT[:, k, :], Wk[:, k, :], k == 0, k == 7, r=["Wk", hTr], w=[("ps", 1)])
                cp("act", ktok2[p_], ps[1], [("ps", 1)], [("ktok", p_)])
                for hh in range(2):
                    b = 2 - hh
                    for k in range(8):
                        mm(ps[b], hT[:, k, :], Wv[:, k, hh * 512:(hh + 1) * 512], k == 0, k == 7,
                           r=["Wv", hTr], w=[("ps", b)])
                    cp("act", vaug2[p_][:, 2 * hh:2 * hh + 2, 0:256], ps[b].rearrange("p (h v) -> p h v", h=2),
                       [("ps", b)], [("vaug", p_)])
                for hh in range(2):
                    b = 2 - hh
                    for k in range(8):
                        mm(ps[b], hT[:, k, :], Wo[:, k, hh * 512:(hh + 1) * 512], k == 0, k == 7,
                           r=["Wo", hTr], w=[("ps", b)])
                    gsl = Gs2[p_][:, hh * 512:(hh + 1) * 512]
                    act(gsl, ps[b], AF.Sigmoid, [("ps", b)], [("Gs", p_, hh)])
                    tt("pool", gsl, gsl, mng[:, hh * 512:(hh + 1) * 512], ALU.mult, [("Gs", p_, hh), "mng"],
                       [("Gs", p_, hh)])
                tt("pool", uv2[p_][:, :, 0:NVA], vaug2[p_][:, :, 0:NVA],
                   Utok[:, c:64:16].unsqueeze(2).to_broadcast([128, 4, NVA]), ALU.mult,
                   [("vaug", p_), "Utok"], [("uv", p_)])

            def stageB1(c):
                p_ = c % 2
                for h in range(4):
                    mm(ps[3][:, h * 128:(h + 1) * 128], kT2[p_][:, h, :], qT2[p_][:, h, :], True, True,
                       r=[("kT", p_), ("qT", p_)], w=[("ps", 3)])
                tt("dve", STb, ps[3].rearrange("p (h t) -> p h t", h=4),
                   maskT[:].unsqueeze(1).to_broadcast([128, 4, 128]), ALU.mult, [("ps", 3), "maskT"], ["STb"])

            def stageB2(c):
                p_ = c % 2
                qT, ktok, uv = qT2[p_], ktok2[p_], uv2[p_]
                for h in range(4):
                    bn = 4 + h
                    mm(ps[bn][:, 0:NVA], STb[:, h, :], uv[:, h, 0:NVA], True, c == 0,
                       r=["STb", ("uv", p_)], w=[("ps", bn)])
                    if c > 0:
                        mm(ps[bn][:, 0:NVA], qT[:, h, :], Cbf[:, h, 0:NVA], False, True,
                           r=[("qT", p_), ("Cbf", h)], w=[("ps", bn)])
                if c < NT - 1:
                    for h in range(4):
                        bp = 1 + (h % 2)
                        col = h * 16 + c
                        mm(ps[bp][:, 0:NVA], ktok[:, h * 128:(h + 1) * 128], uv[:, h, 0:NVA], True, True,
                           r=[("ktok", p_), ("uv", p_)], w=[("ps", bp)])
                        if c == 0:
                            ts("dve", C32[:, h, 0:NVA], ps[bp][:, 0:NVA], DEC[:, col:col + 1], ALU.mult,
                               [("ps", bp), "DEC"], [("C32", h)])
                        else:
                            tt("dve", C32[:, h, 0:NVA], ps[bp][:, 0:NVA], C32[:, h, 0:NVA], ALU.add,
                               [("ps", bp), ("C32", h)], [("C32", h)])
                            act(C32[:, h, 0:NVA], C32[:, h, 0:NVA], AF.Copy, [("C32", h), "DEC"], [("C32", h)],
                                scale=DEC[:, col:col + 1])
                        cp("pool", Cbf[:, h, 0:NVA], C32[:, h, 0:NVA], [("C32", h)], [("Cbf", h)])

            def stageB3(c):
                p_ = c % 2
                Gs = Gs2[p_]
                nres = [("ps", 4 + h) for h in range(4)]
                den4 = psall[:, 4:8, 256]
                cp("dve", s4[:, 4, :], den4, nres, ["s4_4"])
                stt(s4[:, 7, :], s4[:, 4, :], -1.0, s4[:, 4, :], ALU.mult, ALU.max, ["s4_4"], ["s4_7"])
                tt("dve", s4[:, 0, :], s4[:, 7, :], ENtok[:, c:64:16], ALU.max, ["s4_7", "ENtok"], ["s4_0"])
                recip(s4[:, 1, :], s4[:, 0, :], ["s4_0"], ["s4_1"])
                for h in range(4):
                    act(junk[:, 0:256], ps[4 + h][:, 0:256], AF.Square, [("ps", 4 + h), "s4_1"],
                        ["junk", ("s4_2", h)], scale=s4[:, 1, h:h + 1], accum=s4[:, 2, h:h + 1])
                ts("dve", s4[:, 3, :], s4[:, 2, :], 1.0 / 256, ALU.mult, [("s4_2", h) for h in range(4)], ["s4_3"],
                   s2=EPS, op1=ALU.add)
                tt("pool", s4[:, 5, :], s4[:, 3, :], negh[:], ALU.pow, ["s4_3", "negh"], ["s4_5"])
                tt("dve", s4[:, 6, :], s4[:, 5, :], s4[:, 1, :], ALU.mult, ["s4_5", "s4_1"], ["s4_6"])
                for h in range(4):
                    stt(ybf[:, h * 256:(h + 1) * 256], ps[4 + h][:, 0:256], s4[:, 6, h:h + 1],
                        Gs[:, h * 256:(h + 1) * 256], ALU.mult, ALU.mult,
                        [("ps", 4 + h), "s4_6", ("Gs", p_, h // 2)], ["ybf"])

            def stageB4(c):
                for k in range(8):
                    tr(psb[0][:, k, :], ybf[:, k * 128:(k + 1) * 128], ident_b[:], ["ybf"], [("ps", 0)])
                cp("act", yT, psb[0], [("ps", 0)], ["yT"])
                for half in range(2):
                    b = 3 if half == 0 else 0
                    for k in range(8):
                        mm(ps[b], yT[:, k, :], Wout[:, k, half * 512:(half + 1) * 512], k == 0, k == 7,
                           r=["yT", "Wout"], w=[("ps", b)])
                    add_to_x(c, half, ps[b], ("ps", b))

            norm_tile(0, out_bf=hb2[0], res_bf=("hb", 0))
            norm_tile(1, out_bf=hb2[1], res_bf=("hb", 1))
            stageA1(0)
            stageA2(0)
            for c in range(NT):
                stageB1(c)
                if c + 1 < NT:
                    stageA1(c + 1)
                stageB2(c)
                stageB3(c)
                if c + 1 < NT:
                    stageA2(c + 1)
                stageB4(c)
            P.barrier()

        def sconv(j):
            ar.reset()
            Win = ar.alloc([128, 8, 3 * D], BF16)
            Wout = ar.alloc([128, 8, D], BF16)
            cw = ar.alloc([128, 8, 3], F32)
            hTf2 = [ar.alloc([128, 8, 512], BF16) for _ in range(2)]
            hb2 = [hbf[:], ar.alloc([128, D], BF16)]
            U = ar.alloc([128, 8, 516], F32)
            xbs2 = [ar.alloc([128, 512], F32) for _ in range(2)]
            Y2 = [ar.alloc([128, 512], F32) for _ in range(2)]
            zT = ar.alloc([128, 8, 512], BF16)
            for hf_ in range(2):
                for i3 in range(3):
                    c0 = i3 * D + hf_ * 512
                    dma("pool", Win[:, :, c0:c0 + 512],
                        swin_d[j, :, c0:c0 + 512].rearrange("(k p) n -> p k n", p=128), f"w{i3}",
                        r=(), w=[("Win", i3, hf_)])
            dma("pool", Wout, swout_d[j].rearrange("(k p) n -> p k n", p=128), "w3", r=(), w=["Wout"])
            dma("sp", cw, scw_d[j].rearrange("p (c t) -> p c t", t=3), "sm0", r=(), w=["cw"])
            memset("pool", U[:, :, 0:2], 0.0, [("U", f) for f in range(8)])

            def convA(n):
                p_ = n % 2
                for t4 in range(4):
                    t = n * 4 + t4
                    q_ = t4 % 2
                    norm_tile(t, out_bf=hb2[q_], res_bf=("hb", q_))
                    for k in range(8):
                        tr(psb[7][:, k, :], hb2[q_][:, k * 128:(k + 1) * 128], ident_b[:], [("hb", q_)], [("ps", 7)])
                    cp("act", hTf2[p_][:, :, t4 * 128:(t4 + 1) * 128], psb[7], [("ps", 7)], [("hTf", p_)])

            def convB(n):
                p_ = n % 2
                hTf = hTf2[p_]
                for f in range(8):
                    q_ = f % 2
                    bs = q_ * 3
                    xbs, Y = xbs2[q_], Y2[q_]
                    for i3 in (2, 1, 0):
                        for k in range(8):
                            mm(ps[bs + i3], Win[:, k, i3 * D + f * 128:i3 * D + (f + 1) * 128], hTf[:, k, :],
                               k == 0, k == 7, r=[("Win", i3, f // 4), ("hTf", p_)], w=[("ps", bs + i3)])
                    cp("act", xbs, ps[bs + 2], [("ps", bs + 2)], [("xbs", q_)])
                    tt("dve", U[:, f, 2:514], ps[bs + 1], xbs, ALU.mult, [("ps", bs + 1), ("xbs", q_)], [("U", f)])
                    ts("dve", Y, U[:, f, 2:514], cw[:, f, 2:3], ALU.mult, [("U", f), "cw"], [("Y", q_)])
                    stt(Y, U[:, f, 1:513], cw[:, f, 1:2], Y, ALU.mult, ALU.add, [("U", f), "cw", ("Y", q_)], [("Y", q_)])
                    stt(Y, U[:, f, 0:512], cw[:, f, 0:1], Y, ALU.mult, ALU.add, [("U", f), "cw", ("Y", q_)], [("Y", q_)])
                    tt("dve", zT[:, f, :], ps[bs], Y, ALU.mult, [("ps", bs), ("Y", q_)], ["zT"])
                    cp("pool", U[:, f, 0:2], U[:, f, 512:514], [("U", f)], [("U", f)])
                for t4 in range(4):
                    t = n * 4 + t4
                    for half in range(2):
                        b = 6 + half
                        for k in range(8):
                            mm(ps[b], zT[:, k, t4 * 128:(t4 + 1) * 128], Wout[:, k, half * 512:(half + 1) * 512],
                               k == 0, k == 7, r=["zT", "Wout"], w=[("ps", b)])
                        add_to_x(t, half, ps[b], ("ps", b))

            convA(0)
            for n in range(4):
                if n + 1 < 4:
                    convA(n + 1)
                convB(n)
            P.barrier()

        def moe(i, next_sub=None):
            ar.reset()
            DEST = ar.alloc([128, NT, 2], I32)
            WTS = ar.alloc([128, NT, 2], F32)
            mark = ar.off
            Wg = [ar.alloc([128, 8, 512], BF16) for _ in range(2)]
            Wu = [ar.alloc([128, 8, 512], BF16) for _ in range(2)]
            Wd = [ar.alloc([128, 4, D], BF16) for _ in range(2)]
            wend = ar.off
            ar.off = mark
            Wr = ar.alloc([128, 8, 36], F32)
            rb = ar.alloc([128, 36], F32)
            hfs = [ar.alloc([128, D], F32) for _ in range(2)]
            hTfs = [ar.alloc([128, 8, 128], F32) for _ in range(2)]
            ar_small_end = ar.off
            ar.off = max(ar.off, wend)
            HB = ar.alloc([128, NT, D], BF16)
            ar.off = ar_small_end
            LA = ar.alloc([128, NT, 36], F32)
            G4 = [ar.alloc([128, NT, 4], F32) for _ in range(4)]
            Lm = ar.alloc([128, NT, 32], F32)
            Lm2 = ar.alloc([128, NT, 32], F32)
            M1 = ar.alloc([128, NT, 32], F32)
            M2 = ar.alloc([128, NT, 32], F32)
            E = ar.alloc([128, NT, 32], F32)
            PRE = ar.alloc([128, NT, 32], F32)
            CNT = ar.alloc([128, NT, 32], F32)
            JK = ar.alloc([128, NT, 32], F32)
            SV = ar.alloc([128, 28, NT], F32)
            assert ar.off <= wend, ("routing scratch overruns weight slots", ar.off, wend)

            dma("sp", Wr, rw_d[i].rearrange("(k p) n -> p k n", p=128), "sm0", r=(), w=["Wr"])
            dma("sp", rb, rb_d[i, :].partition_broadcast(128), "sm1", r=(), w=["rb"])

            def load_expert(e):
                s_ = e % 2
                g = i * NE + e
                P.add("pool", lambda e, s_=s_, g=g: e.dma_start(
                    out=Wg[s_], in_=ewg_d[g].rearrange("(p k) n -> p k n", p=128), max_dma_last_dim=8192),
                    r=(), w=[("Wg", s_)], dma=f"w{s_}")
                P.add("pool", lambda e, s_=s_, g=g: e.dma_start(
                    out=Wu[s_], in_=ewu_d[g].rearrange("(p k) n -> p k n", p=128), max_dma_last_dim=8192),
                    r=(), w=[("Wu", s_)], dma=f"w{2 + s_}")
                dma("pool", Wd[s_], ewd_d[g].rearrange("(k p) n -> p k n", p=128), f"w{4 + s_}", r=(), w=[("Wd", s_)])

            def r1_norm(t):
                p_ = t % 2
                norm_tile(t, out_f=hfs[p_], res_f=("hf", p_))
                cp("pool", HB[:, t, :].rearrange("q (k p) -> q p k", p=128),
                   hfs[p_].rearrange("q (p k) -> q p k", k=8), [("hf", p_)], [("HB", t)])

            def r1_router(t):
                p_ = t % 2
                hf = hfs[p_]
                hTf = hTfs[p_]
                for g4 in range(2):
                    bk = p_ * 2 + g4
                    for k4 in range(4):
                        k = g4 * 4 + k4
                        tr(ps[bk][:, k4 * 128:(k4 + 1) * 128], hf[:, k * 128:(k + 1) * 128], ident_f[:],
                           [("hf", p_)], [("ps", bk)])
                    cp("act", hTf[:, g4 * 4:g4 * 4 + 4, :],
                       ps[bk].rearrange("p (k m) -> p k m", k=4), [("ps", bk)], [("hTf", p_, g4)])
                bl = 4 + p_
                for k in range(8):
                    mm(ps[bl][:, 0:36], hTf[:, k, :], Wr[:, k, :], k == 0, k == 7,
                       r=[("hTf", p_, k // 4), "Wr"], w=[("ps", bl)])
                tt("dve", LA[:, t, :], ps[bl][:, 0:36], rb, ALU.add, [("ps", bl), "rb"], ["LA"])

            r1_norm(0)
            for t in range(NT):
                if t + 1 < NT:
                    r1_norm(t + 1)
                r1_router(t)

            def bc(v, n):
                return v.unsqueeze(2).to_broadcast([128, NT, n])

            def red(out, in_, op, r, w):
                P.add("dve", lambda e: e.tensor_reduce(out=out, in_=in_, axis=AX.X, op=op), r=r, w=w)

            LA4 = LA[:, :, 0:4]
            LAE = LA[:, :, 4:36]
            red(SV[:, 0, :], LA4, ALU.max, ["LA"], ["gmax"])
            tt("dve", G4[0], LA4, bc(SV[:, 0, :], 4), ALU.subtract, ["LA", "gmax"], ["G40"])
            act(G4[1], G4[0], AF.Exp, ["G40"], ["G41"])
            red(SV[:, 1, :], G4[1], ALU.add, ["G41"], ["gsum"])
            recip(SV[:, 2, :], SV[:, 1, :], ["gsum"], ["psel"])
            tt("dve", G4[2], LA4, bc(SV[:, 0, :], 4), ALU.is_equal, ["LA", "gmax"], ["G42"])
            ts("dve", G4[3], G4[2], -1.0, ALU.add, ["G42"], ["G43"], s2=1.0e30, op1=ALU.mult)
            tt("dve", Lm.rearrange("p t (g e) -> p t g e", g=4), LAE.rearrange("p t (g e) -> p t g e", g=4),
               G4[3].unsqueeze(3).to_broadcast([128, NT, 4, 8]), ALU.add, ["LA", "G43"], ["Lm"])
            red(SV[:, 3, :], Lm, ALU.max, ["Lm"], ["m1"])
            tt("dve", M1, Lm, bc(SV[:, 3, :], 32), ALU.is_equal, ["Lm", "m1"], ["M1"])
            stt(Lm2, M1, -1.0e30, Lm, ALU.mult, ALU.add, ["M1", "Lm"], ["Lm2"])
            red(SV[:, 4, :], Lm2, ALU.max, ["Lm2"], ["m2"])
            tt("dve", M2, Lm2, bc(SV[:, 4, :], 32), ALU.is_equal, ["Lm2", "m2"], ["M2"])
            iob = iota_f[:].unsqueeze(1).to_broadcast([128, NT, 32])
            tt("dve", JK, M1, iob, ALU.mult, ["M1", "iota_f"], ["JK"])
            red(SV[:, 5, :], JK, ALU.add, ["JK"], ["eid0"])
            tt("dve", JK, M2, iob, ALU.mult, ["M2", "iota_f"], ["JK"])
            red(SV[:, 6, :], JK, ALU.add, ["JK"], ["eid1"])
            tt("dve", SV[:, 7, :], SV[:, 4, :], SV[:, 3, :], ALU.subtract, ["m1", "m2"], ["d"])
            act(SV[:, 8, :], SV[:, 7, :], AF.Exp, ["d"], ["e"])
            ts("dve", SV[:, 9, :], SV[:, 8, :], 1.0, ALU.add, ["e"], ["e1"])
            recip(SV[:, 10, :], SV[:, 9, :], ["e1"], ["r"])
            tt("dve", SV[:, 11, :], SV[:, 10, :], SV[:, 2, :], ALU.mult, ["r", "psel"], ["w0"])
            tt("dve", SV[:, 12, :], SV[:, 2, :], SV[:, 11, :], ALU.subtract, ["psel", "w0"], ["w1"])
            tt("dve", E, M1, M2, ALU.add, ["M1", "M2"], ["E"])
            Ef = E.rearrange("p t e -> p (t e)")
            mm(ps[6], Lt[:], Ef, True, True, r=["Lt", "E"], w=[("ps", 6)])
            mm(ps[7], ones_f[:], Ef, True, True, r=["ones_f", "E"], w=[("ps", 7)])
            memset("dve", PRE[:, 0, :], 0.0, ["PRE"])
            for t in range(1, NT):
                tt("dve", PRE[:, t, :], PRE[:, t - 1, :], ps[7][:, (t - 1) * 32:t * 32], ALU.add,
                   ["PRE", ("ps", 7)], ["PRE"])
            tt("dve", CNT.rearrange("p t e -> p (t e)"), PRE.rearrange("p t e -> p (t e)"), ps[6], ALU.add,
               ["PRE", ("ps", 6)], ["CNT"])
            for kk, MM in ((0, M1), (1, M2)):
                tt("dve", JK, MM, CNT, ALU.mult, ["M1", "M2", "CNT"], ["JK"])
                red(SV[:, 13 + kk, :], JK, ALU.add, ["JK"], [("pos", kk)])
                stt(SV[:, 15 + kk, :], SV[:, 5 + kk, :], float(CAP), SV[:, 13 + kk, :], ALU.mult, ALU.add,
                    ["eid0", "eid1", ("pos", kk)], [("dst", kk)])
                ts("dve", SV[:, 17 + kk, :], SV[:, 13 + kk, :], float(CAP), ALU.is_ge, [("pos", kk)], [("ov", kk)],
                   s2=1.0e6, op1=ALU.mult)
                tt("dve", SV[:, 15 + kk, :], SV[:, 15 + kk, :], SV[:, 17 + kk, :], ALU.add,
                   [("dst", kk), ("ov", kk)], [("dst", kk)])
                cp("dve", DEST[:, :, kk], SV[:, 15 + kk, :], [("dst", kk)], ["DEST"])
                ts("dve", SV[:, 19 + kk, :], SV[:, 13 + kk, :], float(CAP), ALU.is_lt, [("pos", kk)], [("keep", kk)])
                tt("dve", WTS[:, :, kk], SV[:, 11 + kk, :], SV[:, 19 + kk, :], ALU.mult,
                   ["w0", "w1", ("keep", kk)], ["WTS"])

            P._merge(P.pending["pool"], ("d", "xdz", P.dma_cnt["xdz"]), "raw")
            for t in range(NT):
                for kk in range(2):
                    P.add("pool", lambda e, t=t, kk=kk: e.indirect_dma_start(
                        out=Xd, out_offset=bass.IndirectOffsetOnAxis(ap=DEST[:, t, kk:kk + 1], axis=0),
                        in_=HB[:, t, :], in_offset=None, bounds_check=breg[0], oob_is_err=False),
                        r=["DEST", ("HB", t)], w=[("Xd", t, kk)], dma=f"sc{kk}")
            P.barrier()

            ar.off = wend
            Xtok = [ar.alloc([128, NJ, D], BF16) for _ in range(2)]
            XeT = [ar.alloc([128, 8, CAP], BF16) for _ in range(2)]
            sg = [ar.alloc([128, CAP], F32) for _ in range(2)]
            AT = [ar.alloc([128, 4, CAP], BF16) for _ in range(2)]
            yo_lo = ar.off
            Yo = [ar.alloc([128, D], BF16) for _ in range(4)]

            def x_load(e):
                s_ = e % 2
                dma("sp", Xtok[s_], Xd[e * CAP:(e + 1) * CAP, :].rearrange("(j p) d -> p j d", p=128), f"xl{s_}",
                    r=(), w=[("Xtok", s_)])

            def t_phase(e):
                s_ = e % 2
                for jj in range(NJ):
                    b = jj % 2
                    for k in range(8):
                        tr(psb[b][:, k, :], Xtok[s_][:, jj, k * 128:(k + 1) * 128], ident_b[:],
                           [("Xtok", s_)], [("ps", b)])
                    cp("act" if jj % 2 == 0 else "dve", XeT[s_][:, :, jj * 128:(jj + 1) * 128], psb[b],
                       [("ps", b)], [("XeT", s_, jj)])

            def gu_phase(e):
                s_ = e % 2
                xr = [("XeT", s_, jj) for jj in range(NJ)]
                for f in range(4):
                    bg = 2 + (f % 2) * 2
                    bu = bg + 1
                    for k in range(8):
                        mm(ps[bg], Wg[s_][:, k, f * 128:(f + 1) * 128], XeT[s_][:, k, :], k == 0, k == 7,
                           r=[("Wg", s_)] + xr, w=[("ps", bg)])
                    for k in range(8):
                        mm(ps[bu], Wu[s_][:, k, f * 128:(f + 1) * 128], XeT[s_][:, k, :], k == 0, k == 7,
                           r=[("Wu", s_)] + xr, w=[("ps", bu)])
                    act(sg[f % 2], ps[bg], AF.Silu, [("ps", bg)], [("sg", f % 2)])
                    tt("dve", AT[s_][:, f, :], ps[bu], sg[f % 2], ALU.mult, [("ps", bu), ("sg", f % 2)],
                       [("AT", s_, f)])

            yo_ctr = [0]

            def d_phase(e):
                s_ = e % 2
                ar_ = [("AT", s_, f) for f in range(4)]
                for jj in range(NJ):
                    yi = yo_ctr[0] % 4
                    yo_ctr[0] += 1
                    yo = Yo[yi]
                    yor = ("Yo", yi)
                    for half in range(2):
                        b = 6 + half
                        for f in range(4):
                            mm(ps[b], AT[s_][:, f, jj * 128:(jj + 1) * 128], Wd[s_][:, f, half * 512:(half + 1) * 512],
                               f == 0, f == 3, r=[("Wd", s_)] + ar_, w=[("ps", b)])
                        tt("dve", yo[:, half * 512:(half + 1) * 512], ps[b], modG[:, half * 512:(half + 1) * 512],
                           ALU.mult, [("ps", b), "modG"], [yor])
                    dma("sp", Yd[e * CAP + jj * 128:e * CAP + (jj + 1) * 128, :], yo, f"yo{yi}",
                        r=[yor], w=[("Yd", e, jj)])

            x_load(0)
            x_load(1)
            load_expert(0)
            t_phase(0)
            for e_ in range(NE):
                if e_ + 2 < NE:
                    x_load(e_ + 2)
                if e_ + 1 < NE:
                    load_expert(e_ + 1)
                gu_phase(e_)
                if e_ + 1 < NE:
                    t_phase(e_ + 1)
                d_phase(e_)
            P.barrier()

            nxt = None
            if next_sub is not None:
                yo_off = ar.off
                nxt = adaln_issue(next_sub, ng_d[next_sub, :], base=mark)
                assert ar.off <= yo_lo, ("adaLN prefetch overlaps live combine buffers", ar.off, yo_lo)
                ar.off = yo_off
            for yi in range(4):
                memset("pool", Yo[yi], 0.0, [("Yo", yi)])

            def gathers(t):
                for kk in range(2):
                    yi = (t % 2) * 2 + kk
                    P.add("pool", lambda e, t=t, kk=kk, yi=yi: e.indirect_dma_start(
                        out=Yo[yi][:], out_offset=None, in_=Yd,
                        in_offset=bass.IndirectOffsetOnAxis(ap=DEST[:, t, kk:kk + 1], axis=0),
                        bounds_check=breg[0], oob_is_err=False),
                        r=["DEST"], w=[("Yo", yi)], dma=f"ga{yi}")

            gathers(0)
            for t in range(NT):
                if t + 1 < NT:
                    gathers(t + 1)
                y0, y1 = (t % 2) * 2, (t % 2) * 2 + 1
                stt(xres[:, t, :], Yo[y0], WTS[:, t, 0:1], xres[:, t, :], ALU.mult, ALU.add,
                    [("Yo", y0), "WTS", ("x", t)], [("x", t)])
                stt(xres[:, t, :], Yo[y1], WTS[:, t, 1:2], xres[:, t, :], ALU.mult, ALU.add,
                    [("Yo", y1), "WTS", ("x", t)], [("x", t)])
            if nxt is not None:
                adaln_finish(nxt)
                return
            P.barrier()

        for t4 in range(4):
            dma("sp", xres[:, t4 * 4:(t4 + 1) * 4, :],
                x_d[t4 * 512:(t4 + 1) * 512, :].rearrange("(t p) d -> p t d", p=128),
                "xin", r=(), w=[("x", t) for t in range(t4 * 4, t4 * 4 + 4)])
        dma("sp", cs[:], c_d, "cin", r=(), w=["cs"])
        pre0 = [None]
        breg = [None]

        def _mk_breg(e):
            breg[0] = e.to_reg(NE * CAP - 1)
            return e.memset(ident_f[:], 0.0)
        P.add("pool", _mk_breg, r=(), w=["ident_f"])
        P.add("pool", lambda e: e.affine_select(out=ident_f[:], in_=ident_f[:], pattern=[[-1, 128]], base=0,
                                                channel_multiplier=1, compare_op=ALU.not_equal, fill=1.0),
              r=["ident_f"], w=["ident_f"])
        cp("pool", ident_b[:], ident_f[:], ["ident_f"], ["ident_b"])
        memset("pool", maskT[:], 1.0, ["maskT"])
        P.add("pool", lambda e: e.affine_select(out=maskT[:], in_=maskT[:], pattern=[[1, 128]], base=0,
                                                channel_multiplier=-1, compare_op=ALU.is_ge, fill=0.0),
              r=["maskT"], w=["maskT"])
        memset("pool", Lt[:], 1.0, ["Lt"])
        P.add("pool", lambda e: e.affine_select(out=Lt[:], in_=Lt[:], pattern=[[1, 128]], base=-1,
                                                channel_multiplier=-1, compare_op=ALU.is_ge, fill=0.0),
              r=["Lt"], w=["Lt"])
        memset("pool", ones_f[:], 1.0, ["ones_f"])
        memset("pool", TT[:], 1.0, ["TT"])
        P.add("pool", lambda e: e.affine_select(out=TT[:], in_=TT[:], pattern=[[1, 64]], base=-1,
                                                channel_multiplier=-1, compare_op=ALU.is_ge, fill=0.0),
              r=["TT"], w=["TT"])
        for hb in range(1, 4):
            P.add("pool", lambda e, hb=hb: e.affine_select(out=TT[:, 16 * hb:16 * hb + 16],
                                                           in_=TT[:, 16 * hb:16 * hb + 16],
                                                           pattern=[[0, 16]], base=-16 * hb, channel_multiplier=1,
                                                           compare_op=ALU.is_ge, fill=0.0),
                  r=["TT"], w=["TT"])
        P.add("pool", lambda e: e.iota(iota_i[:], pattern=[[1, 32]], base=0, channel_multiplier=0),
              r=(), w=["iota_i"])
        cp("pool", iota_f[:], iota_i[:], ["iota_i"], ["iota_f"])
        act(sc[:], cs[:], AF.Silu, ["cs"], ["sc"])
        memset("pool", negh[:], -0.5, ["negh"])
        pre0[0] = adaln_issue(0, ng_d[0, :])
        P.bg_keys.add("xdz")
        memset("pool", ztile, 0.0, ["ztile"])
        Xdv = Xd.rearrange("(a p r) (h c) -> a p (r h) c", p=128, r=8, h=2)
        for a_ in range(16):
            xdz_ev[0] = P.add("sp", lambda e, a_=a_: e.dma_start(
                out=Xdv[a_], in_=ztile.unsqueeze(1).to_broadcast([128, 16, 512])),
                r=["ztile"], w=[("Xdz", a_)], dma="xdz")
        P.barrier()


        pre_done = False
        for sub in range(n_sub):
            i, s_ = sub // 2, sub % 2
            if not pre_done:
                if sub == 0:
                    adaln_finish(pre0[0])
                else:
                    adaln(sub, ng_d[sub, :])
            pre_done = False
            if s_ == 0:
                if i % 2 == 0:
                    mlstm(i // 2)
                else:
                    sconv(i // 2)
            else:
                if sub + 1 < n_sub:
                    moe(i, next_sub=sub + 1)
                    pre_done = True
                else:
                    moe(i)

        ar.reset()
        if do_final:
            dma("sp", modA[:], fg_d[0, :].partition_broadcast(128), "gbc", r=(), w=["modA"])
            compute_rstd()
        outs = [ar.alloc([128, D], F32) for _ in range(2)]
        for t in range(NT):
            o = outs[t % 2]
            if do_final:
                norm_tile(t, out_f=o, shift=False)
                dma("sp", y_d[t * 128:(t + 1) * 128, :], o, f"out{t % 2}", r=["hf"], w=[("y", t)])
            else:
                dma("sp", y_d[t * 128:(t + 1) * 128, :], xres[:, t, :], f"out{t % 2}", r=[("x", t)], w=[("y", t)])

        dma_keys = sorted(P.dma_cnt.keys())
        eng_sems = {e: es.enter_context(nc.semaphore(f"s_{e}")) for e in ENGS}
        dma_sems = {k: es.enter_context(nc.semaphore(f"d_{k}")) for k in dma_keys}
        bodies = P.emit(None, eng_sems, dma_sems)
        with nc.Block() as block:
            block.tensor(bodies["pe"])
            block.scalar(bodies["act"])
            block.vector(bodies["dve"])
            block.gpsimd(bodies["pool"])
            block.sync(bodies["sp"])
        n_ops = {e: len(P.ops[e]) for e in ENGS}
    return nc, n_ops


def make_in_maps(inputs):
    f = lambda a: np.ascontiguousarray(np.asarray(a, dtype=np.float32))
    x = f(inputs["x"])
    c = f(inputs["c"])
    B = x.shape[0]
    shared = {
        "ada_w": f(inputs["ada_w"]).reshape(2 * DEPTH, D, 3 * D),
        "ada_b": f(inputs["ada_b"]).reshape(2 * DEPTH, 3 * D),
        "norm_g": f(inputs["norm_g"]).reshape(2 * DEPTH, D),
        "final_g": f(inputs["final_g"]).reshape(1, D),
        "m_w_in": f(inputs["m_w_in"]),
        "m_b_gates": f(inputs["m_b_gates"]),
        "m_norm_g": f(inputs["m_norm_g"]),
        "m_w_out": f(inputs["m_w_out"]),
        "s_w_in": f(inputs["s_w_in"]),
        "s_conv_w": np.ascontiguousarray(
            f(inputs["s_conv_w"]).reshape(2, 3, 8, 128).transpose(0, 3, 2, 1).reshape(2, 128, 24)),
        "s_w_out": f(inputs["s_w_out"]),
        "r_w": np.ascontiguousarray(np.concatenate([f(inputs["r_w_group"]), f(inputs["r_w_expert"])], axis=-1)),
        "r_b": np.ascontiguousarray(np.concatenate([f(inputs["r_b_group"]), f(inputs["r_b_expert"])], axis=-1)),
        "e_w_gate": f(inputs["e_w_gate"]).reshape(DEPTH * NE, D, 512),
        "e_w_up": f(inputs["e_w_up"]).reshape(DEPTH * NE, D, 512),
        "e_w_down": f(inputs["e_w_down"]).reshape(DEPTH * NE, 512, D),
    }
    maps = []
    for b in range(B):
        m = dict(shared)
        m["x"] = np.ascontiguousarray(x[b])
        m["c"] = np.ascontiguousarray(c[b].reshape(8, 128).T)
        maps.append(m)
    return maps


def kernel(**inputs):
    nc, _ = build_program()
    in_maps = make_in_maps(inputs)
    res = run_bass_kernel_spmd(nc, in_maps, core_ids=list(range(len(in_maps))))
    return np.stack([r["y"] for r in res.results], axis=0).astype(np.float32)
```

```python
import numpy as np
import concourse.bass as bass
import concourse.mybir as mybir
from concourse.bass_utils import run_bass_kernel_spmd

F32 = mybir.dt.float32
BF16 = mybir.dt.bfloat16
I32 = mybir.dt.int32
U32 = mybir.dt.uint32
AF = mybir.ActivationFunctionType
ALU = mybir.AluOpType
AX = mybir.AxisListType

S = 2048
D = 1024
NT = 16
DEPTH = 4
NH = 4
DVA = 258
NVA = 257
NE = 32
CAP = 512
NJ = CAP // 128
EPS = 1e-6
ENGS = ("pe", "act", "dve", "pool", "sp")


class Prog:
    def __init__(self, nc):
        self.nc = nc
        self.ops = {e: [] for e in ENGS}
        self.last_w = {}
        self.readers = {}
        self.dma_cnt = {}
        self.pending = {e: {} for e in ENGS}
        self.last_compute = {e: None for e in ENGS}
        self.bg_keys = set()

    @staticmethod
    def _merge(dst, ev, kind):
        k = ev[:2]
        old = dst.get(k)
        if old is None:
            old = [None, None]
            dst[k] = old
        if old[0] is None or old[0][2] < ev[2]:
            old[0] = ev
        if kind != "war" and (old[1] is None or old[1][2] < ev[2]):
            old[1] = ev

    def add(self, eng, fn, r=(), w=(), dma=None):
        idx = len(self.ops[eng])
        deps = {}
        for k, v in self.pending[eng].items():
            deps[k] = list(v)
        self.pending[eng] = {}
        for res in r:
            ev = self.last_w.get(res)
            if ev is not None:
                self._merge(deps, ev, "raw")
        for res in w:
            ev = self.last_w.get(res)
            if ev is not None:
                self._merge(deps, ev, "waw")
            for ev2 in self.readers.get(res, {}).values():
                self._merge(deps, ev2, "war")
        if dma is not None:
            self.dma_cnt[dma] = self.dma_cnt.get(dma, 0) + 16
            myev = ("d", dma, self.dma_cnt[dma])
        else:
            myev = ("c", eng, idx)
            self.last_compute[eng] = myev
        need = []
        for ev_any, ev_nw in deps.values():
            if ev_any[0] == "c" and ev_any[1] == eng and dma is None and eng == "pe":
                continue
            need.append(ev_any)
        self.ops[eng].append(dict(fn=fn, deps=need, ev=myev, dma=dma))
        for res in r:
            self.readers.setdefault(res, {})[myev[:2]] = myev
        for res in w:
            self.last_w[res] = myev
            self.readers[res] = {}
        return myev

    def barrier(self):
        evs = []
        for e in ENGS:
            if self.last_compute[e] is not None:
                evs.append(self.last_compute[e])
        for k, c in self.dma_cnt.items():
            if k not in self.bg_keys:
                evs.append(("d", k, c))
        for e in ENGS:
            for ev in evs:
                self._merge(self.pending[e], ev, "raw")
        self.last_w = {}
        self.readers = {}

    def emit(self, handles, eng_sems, dma_sems):
        sig = {e: set() for e in ENGS}
        for e in ENGS:
            for op in self.ops[e]:
                for ev in op["deps"]:
                    if ev[0] == "c":
                        sig[ev[1]].add(ev[2])
        rank = {}
        for e in ENGS:
            n = 0
            rank[e] = {}
            for i in range(len(self.ops[e])):
                if i in sig[e]:
                    n += 1
                    rank[e][i] = n

        def run(eng):
            def body(h):
                waited = {}
                for i, op in enumerate(self.ops[eng]):
                    for ev in op["deps"]:
                        if ev[0] == "c":
                            key = ("c", ev[1])
                            val = rank[ev[1]][ev[2]]
                            sem = eng_sems[ev[1]]
                        else:
                            key = ("d", ev[1])
                            val = ev[2]
                            sem = dma_sems[ev[1]]
                        if waited.get(key, 0) >= val:
                            continue
                        h.wait_ge(sem, val)
                        waited[key] = val
                    inst = op["fn"](h)
                    if op["dma"] is not None:
                        inst.then_inc(dma_sems[op["dma"]], 16)
                    elif i in sig[eng]:
                        inst.then_inc(eng_sems[eng], 1)
                if eng == "sp":
                    for k, c in self.dma_cnt.items():
                        if waited.get(("d", k), 0) < c:
                            h.wait_ge(dma_sems[k], c)
            return body
        return {e: run(e) for e in ENGS}


class Arena:
    def __init__(self, t, nbytes):
        self.t = t
        self.n = nbytes
        self.off = 0

    def reset(self):
        self.off = 0

    def alloc(self, shape, dt):
        esz = 4 if dt in (F32, I32, U32) else 2
        per = 1
        for s in shape[1:]:
            per *= s
        nb = (per * esz + 63) // 64 * 64
        assert self.off + nb <= self.n - 1024, ("arena overflow", self.off, nb, self.n)
        v = self.t[:, self.off // 4:(self.off + nb) // 4]
        self.off += nb
        if dt != F32:
            v = v.bitcast(dt)
        v = v[0:shape[0], 0:per]
        if len(shape) == 3:
            v = v.rearrange("p (a b) -> p a b", a=shape[1])
        elif len(shape) == 4:
            v = v.rearrange("p (a b c) -> p a b c", a=shape[1], b=shape[2])
        return v


def build_program(n_sub=2 * DEPTH, do_final=True):
    nc = bass.Bass("TRN2", target_bir_lowering=False)

    def din(name, shape, dt=F32):
        return nc.dram_tensor(name, shape, dt, kind="ExternalInput").ap()

    x_d = din("x", [S, D])
    c_d = din("c", [128, 8])
    adaw_d = din("ada_w", [2 * DEPTH, D, 3 * D])
    adab_d = din("ada_b", [2 * DEPTH, 3 * D])
    ng_d = din("norm_g", [2 * DEPTH, D])
    fg_d = din("final_g", [1, D])
    mwin_d = din("m_w_in", [2, D, 3080])
    mbg_d = din("m_b_gates", [2, 8])
    mng_d = din("m_norm_g", [2, D])
    mwout_d = din("m_w_out", [2, D, D])
    swin_d = din("s_w_in", [2, D, 3 * D])
    scw_d = din("s_conv_w", [2, 128, 24])
    swout_d = din("s_w_out", [2, D, D])
    rw_d = din("r_w", [DEPTH, D, 36])
    rb_d = din("r_b", [DEPTH, 36])
    ewg_d = din("e_w_gate", [DEPTH * NE, D, 512])
    ewu_d = din("e_w_up", [DEPTH * NE, D, 512])
    ewd_d = din("e_w_down", [DEPTH * NE, 512, D])
    y_d = nc.dram_tensor("y", [S, D], F32, kind="ExternalOutput").ap()
    Xd = nc.dram_tensor("xd_scr", [NE * CAP, D], BF16, kind="Internal").ap()
    Yd = nc.dram_tensor("yd_scr", [NE * CAP, D], BF16, kind="Internal").ap()

    ARENA_BYTES = 116 * 1024
    from contextlib import ExitStack
    with ExitStack() as es:
        def sb(name, shape, dt=F32):
            return es.enter_context(nc.sbuf_tensor(name, shape, dt))

        xres = sb("xres", [128, NT, D])
        modA = sb("modA", [128, D])
        modB = sb("modB", [128, D])
        modG = sb("modG", [128, D])
        ident_f = sb("ident_f", [128, 128])
        ident_b = sb("ident_b", [128, 128], BF16)
        maskT = sb("maskT", [128, 128])
        Lt = sb("Lt", [128, 128])
        ones_f = sb("ones_f", [128, 128])
        TT = sb("TT", [64, 64])
        iota_i = sb("iota_i", [128, 32], I32)
        iota_f = sb("iota_f", [128, 32])
        cs = sb("cs", [128, 8])
        sc = sb("sc", [128, 8])
        junk = sb("junk", [128, D], BF16)
        tmpf = sb("tmpf", [128, D])
        hbf = sb("hbf", [128, D], BF16)
        st = sb("st", [128, 16])
        RSTD = sb("RSTD", [128, NT])
        SSt = sb("SSt", [128, NT])
        ntmp = sb("ntmp", [128, D])
        arena_t = sb("arena", [128, ARENA_BYTES // 4])
        ar = Arena(arena_t, ARENA_BYTES)
        ztile = arena_t[:, (ARENA_BYTES - 1024) // 4:ARENA_BYTES // 4].bitcast(BF16)
        xdz_ev = [None]
        psall = es.enter_context(nc.psum_tensor("psall", [128, 8, 512], F32))
        ps = [psall[:, i, :] for i in range(8)]
        psb = [psall[:, i, :].bitcast(BF16).rearrange("p (k m) -> p k m", k=8) for i in range(8)]
        negh = sb("negh", [128, 4])

        P = Prog(nc)

        def mm(out, lhsT, rhs, start, stop, r, w):
            P.add("pe", lambda e: e.matmul(out, lhsT=lhsT, rhs=rhs, start=start, stop=stop), r=r, w=w)

        def tr(out, in_, ident, r, w):
            P.add("pe", lambda e: e.transpose(out=out, in_=in_, identity=ident), r=r, w=w)

        def act(out, in_, func, r, w, scale=1.0, bias=0.0, accum=None, eng="act"):
            P.add(eng, lambda e: e.activation(out=out, in_=in_, func=func, bias=bias, scale=scale,
                                              accum_out=accum), r=r, w=w)

        def tt(eng, out, in0, in1, op, r, w):
            P.add(eng, lambda e: e.tensor_tensor(out=out, in0=in0, in1=in1, op=op), r=r, w=w)

        def ts(eng, out, in0, s1, op0, r, w, s2=None, op1=None):
            if op1 is None:
                P.add(eng, lambda e: e.tensor_scalar(out=out, in0=in0, scalar1=s1, scalar2=None, op0=op0), r=r, w=w)
            else:
                P.add(eng, lambda e: e.tensor_scalar(out=out, in0=in0, scalar1=s1, scalar2=s2, op0=op0, op1=op1),
                      r=r, w=w)

        def stt(out, in0, scalar, in1, op0, op1, r, w):
            P.add("dve", lambda e: e.scalar_tensor_tensor(out=out, in0=in0, scalar=scalar, in1=in1,
                                                          op0=op0, op1=op1), r=r, w=w)

        def cp(eng, out, in_, r, w):
            if eng == "act":
                P.add("act", lambda e: e.copy(out=out, in_=in_), r=r, w=w)
            else:
                P.add(eng, lambda e: e.tensor_copy(out=out, in_=in_), r=r, w=w)

        def recip(out, in_, r, w):
            P.add("dve", lambda e: e.reciprocal(out=out, in_=in_), r=r, w=w)

        def dma(q, out, in_, key, r, w):
            P.add(q, lambda e: e.dma_start(out=out, in_=in_), r=r, w=w, dma=key)

        def memset(eng, ap, val, w):
            P.add(eng, lambda e: e.memset(ap, val), r=(), w=w)

        def compute_rstd():
            for t in range(NT):
                act(junk[:], xres[:, t, :], AF.Square, [("x", t)], ["junk", "SSt"], accum=SSt[:, t:t + 1])
            ts("dve", SSt[:], SSt[:], 1.0 / D, ALU.mult, ["SSt"], ["SSt"], s2=EPS, op1=ALU.add)
            act(SSt[:], SSt[:], AF.Sqrt, ["SSt"], ["SSt"])
            recip(RSTD[:], SSt[:], ["SSt"], ["RSTD"])

        def adaln_issue(sub, g_src, base=0):
            ar.off = base
            bufs = dict(
                SCB=ar.alloc([128, 8, 128], BF16),
                WA=[ar.alloc([128, 8, D], BF16) for _ in range(3)],
                adab=ar.alloc([128, 3 * D], F32),
                gbc=ar.alloc([128, D], F32),
            )
            for w3 in range(3):
                dma("pool", bufs["WA"][w3],
                    adaw_d[sub, :, w3 * D:(w3 + 1) * D].rearrange("(k p) n -> p k n", p=128),
                    f"wa{w3}", r=(), w=[("WA", w3)])
            dma("sp", bufs["adab"], adab_d[sub, :].partition_broadcast(128), "adab", r=(), w=["adab"])
            dma("sp", bufs["gbc"], g_src.partition_broadcast(128), "gbc", r=(), w=["gbc"])
            return bufs

        def adaln_finish(bufs):
            SCB, WA, adab, gbc = bufs["SCB"], bufs["WA"], bufs["adab"], bufs["gbc"]
            cp("dve", SCB, sc[:].unsqueeze(2).to_broadcast([128, 8, 128]), ["sc"], ["SCB"])
            compute_rstd()
            for blk in range(6):
                b = blk % 2
                which = blk // 2
                hcol = (blk % 2) * 512
                for k in range(8):
                    mm(ps[b], SCB[:, k, :], WA[which][:, k, hcol:hcol + 512], k == 0, k == 7,
                       r=["SCB", ("WA", which)], w=[("ps", b)])
                half = slice(hcol, hcol + 512)
                cols = slice(blk * 512, blk * 512 + 512)
                if which == 0:
                    tt("dve", modB[:, half], ps[b], adab[:, cols], ALU.add, [("ps", b), "adab"], ["modB"])
                elif which == 1:
                    tt("dve", tmpf[:, half], ps[b], adab[:, cols], ALU.add, [("ps", b), "adab"], ["tmpf"])
                    stt(modA[:, half], tmpf[:, half], 1.0, gbc[:, half], ALU.add, ALU.mult,
                        ["tmpf", "gbc"], ["modA"])
                else:
                    tt("dve", modG[:, half], ps[b], adab[:, cols], ALU.add, [("ps", b), "adab"], ["modG"])
            P.barrier()

        def adaln(sub, g_src):
            adaln_finish(adaln_issue(sub, g_src))

        def norm_tile(t, out_bf=None, out_f=None, shift=True, res_f="hf", res_bf="hbf"):
            xr = ("x", t)
            if shift:
                stt(ntmp[:], xres[:, t, :], RSTD[:, t:t + 1], modA[:], ALU.mult, ALU.mult,
                    [xr, "RSTD", "modA"], ["ntmp"])
                if out_f is not None:
                    tt("dve", out_f, ntmp[:], modB[:], ALU.add, ["ntmp", "modB"], [res_f])
                    if out_bf is not None:
                        cp("act", out_bf, out_f, [res_f], [res_bf])
                else:
                    tt("dve", out_bf, ntmp[:], modB[:], ALU.add, ["ntmp", "modB"], [res_bf])
            else:
                stt(out_f, xres[:, t, :], RSTD[:, t:t + 1], modA[:], ALU.mult, ALU.mult,
                    [xr, "RSTD", "modA"], [res_f])

        def transpose_tile_bf(src, src_res, dst, dst_res, bank):
            for k in range(8):
                tr(psb[bank][:, k, :], src[:, k * 128:(k + 1) * 128], ident_b[:], [src_res], [("ps", bank)])
            cp("act", dst, psb[bank], [("ps", bank)], [dst_res])

        def add_to_x(t, half, psum_ap, psres):
            hs = slice(half * 512, half * 512 + 512)
            tt("dve", tmpf[:, hs], psum_ap, modG[:, hs], ALU.mult, [psres, "modG"], ["tmpf"])
            tt("pool", xres[:, t, hs], xres[:, t, hs], tmpf[:, hs], ALU.add, [("x", t), "tmpf"], [("x", t)])

        def mlstm(j):
            ar.reset()
            Wq = ar.alloc([128, 8, 512], BF16)
            Wk = ar.alloc([128, 8, 512], BF16)
            Wv = ar.alloc([128, 8, 1024], BF16)
            Wo = ar.alloc([128, 8, 1024], BF16)
            Wgt = ar.alloc([128, 8, 8], BF16)
            Wout = ar.alloc([128, 8, 1024], BF16)
            bgb = ar.alloc([128, 8], F32)
            mng = ar.alloc([128, D], F32)
            hT2 = [ar.alloc([128, 8, 128], BF16) for _ in range(2)]
            hb2 = [hbf[:], ar.alloc([128, D], BF16)]
            Gtok = ar.alloc([128, 2, 64], F32)
            rows = [ar.alloc([64, 128], F32) for _ in range(5)]
            E1, L1, Bn, A_, U_ = rows
            sm = ar.alloc([64, 8], F32)
            rrow = ar.alloc([1, 3, 64], F32)
            MG = ar.alloc([64, 4], F32)
            Utok = ar.alloc([128, 64], F32)
            ENtok = ar.alloc([128, 64], F32)
            DEC = ar.alloc([128, 64], F32)
            qT2 = [ar.alloc([128, 4, 128], BF16) for _ in range(2)]
            kT2 = [ar.alloc([128, 4, 128], BF16) for _ in range(2)]
            ktok2 = [ar.alloc([128, 512], BF16) for _ in range(2)]
            vaug2 = [ar.alloc([128, 4, DVA], BF16) for _ in range(2)]
            uv2 = [ar.alloc([128, 4, DVA], BF16) for _ in range(2)]
            STb = ar.alloc([128, 4, 128], BF16)
            Gs2 = [ar.alloc([128, D], BF16) for _ in range(2)]
            C32 = ar.alloc([128, 4, DVA], F32)
            Cbf = ar.alloc([128, 4, DVA], BF16)
            ybf = ar.alloc([128, D], BF16)
            yT = ar.alloc([128, 8, 128], BF16)
            s4 = ar.alloc([128, 8, 4], F32)

            def wload(dst, src, key, res):
                dma("pool", dst, src.rearrange("(k p) n -> p k n", p=128), key, r=(), w=[res])
            wload(Wgt, mwin_d[j, :, 3072:3080], "w0", "Wgt")
            wload(Wq, mwin_d[j, :, 0:512], "w1", "Wq")
            wload(Wk, mwin_d[j, :, 512:1024], "w2", "Wk")
            wload(Wv, mwin_d[j, :, 1024:2048], "w3", "Wv")
            wload(Wo, mwin_d[j, :, 2048:3072], "w4", "Wo")
            wload(Wout, mwout_d[j], "w5", "Wout")
            dma("sp", bgb, mbg_d[j, :].partition_broadcast(128), "sm0", r=(), w=["bgb"])
            dma("sp", mng, mng_d[j, :].partition_broadcast(128), "sm1", r=(), w=["mng"])
            for p_ in range(2):
                memset("pool", vaug2[p_][:, :, 256:258], 1.0, [("vaug", p_)])

            def pre_norm(t):
                p_ = t % 2
                norm_tile(t, out_bf=hb2[p_], res_bf=("hb", p_))

            def pre_gates(t):
                p_ = t % 2
                bank = 6 + p_
                for k in range(8):
                    tr(psb[bank][:, k, :], hb2[p_][:, k * 128:(k + 1) * 128], ident_b[:], [("hb", p_)], [("ps", bank)])
                cp("act", hT2[p_], psb[bank], [("ps", bank)], [("hT", p_)])
                bg_ = 4 + p_
                for k in range(8):
                    mm(ps[bg_][:, 0:8], hT2[p_][:, k, :], Wgt[:, k, :], k == 0, k == 7,
                       r=[("hT", p_), "Wgt"], w=[("ps", bg_)])
                tt("dve", Gtok[:, :, t:64:16],
                   ps[bg_][:, 0:8].rearrange("p (g h) -> p g h", g=2), bgb[:].rearrange("p (g h) -> p g h", g=2),
                   ALU.add, [("ps", bg_), "bgb"], ["Gtok"])

            pre_norm(0)
            for t in range(NT):
                if t + 1 < NT:
                    pre_norm(t + 1)
                pre_gates(t)
            tr(ps[0][0:64, 0:128], Gtok[:, 0, :], ident_f[:], ["Gtok"], [("ps", 0)])
            tr(ps[1][0:64, 0:128], Gtok[:, 1, :], ident_f[:], ["Gtok"], [("ps", 1)])
            act(E1, ps[1][0:64, 0:128], AF.Exp, [("ps", 1)], ["E1"], scale=-1.0)
            act(L1, E1, AF.Ln, ["E1"], ["L1"], bias=1.0)
            P.add("dve", lambda e: e.tensor_tensor_scan(out=Bn, data0=L1, data1=L1, initial=0.0,
                                                        op0=ALU.add, op1=ALU.max), r=["L1"], w=["Bn"])
            mm(ps[2][0:64, 0:1], TT[:, :], Bn[:, 127:128], True, True, r=["TT", "Bn"], w=[("ps", 2)])
            ts("dve", Bn, Bn, ps[2][0:64, 0:1], ALU.add, ["Bn", ("ps", 2)], ["Bn"])
            tt("dve", A_, ps[0][0:64, 0:128], Bn, ALU.add, [("ps", 0), "Bn"], ["A"])
            P.add("dve", lambda e: e.tensor_reduce(out=sm[:, 0:1], in_=A_, axis=AX.X, op=ALU.max),
                  r=["A"], w=["rmax"])
            tr(ps[3][0:1, 0:64], sm[:, 0:1], ident_f[0:64, 0:64], ["rmax"], [("ps", 3)])
            cp("dve", rrow[:, 0, :], ps[3][0:1, 0:64], [("ps", 3)], ["rrow0"])
            for h in range(4):
                P.add("dve", lambda e, h=h: e.tensor_tensor_scan(
                    out=rrow[:, 1, h * 16:(h + 1) * 16], data0=rrow[:, 0, h * 16:(h + 1) * 16],
                    data1=rrow[:, 0, h * 16:(h + 1) * 16], initial=0.0, op0=ALU.max, op1=ALU.max),
                    r=["rrow0"], w=["rrow1"])
            memset("dve", rrow[:, 2, :], 0.0, ["rrow2"])
            cp("dve", rrow[:, 2, :].rearrange("p (h c) -> p h c", h=4)[:, :, 1:16],
               rrow[:, 1, :].rearrange("p (h c) -> p h c", h=4)[:, :, 0:15], ["rrow1", "rrow2"], ["rrow2"])
            tr(ps[4][0:64, 0:1], rrow[:, 2, :], ident_f[0:1, 0:1], ["rrow2"], [("ps", 4)])
            tr(ps[4][0:64, 1:2], rrow[:, 1, :], ident_f[0:1, 0:1], ["rrow1"], [("ps", 4)])
            cp("dve", MG[:, 0:2], ps[4][0:64, 0:2], [("ps", 4)], ["MG"])
            ts("dve", MG[:, 2:3], MG[:, 0:1], -1.0, ALU.mult, ["MG"], ["MGn"])
            tt("dve", MG[:, 3:4], MG[:, 0:1], MG[:, 1:2], ALU.subtract, ["MG"], ["MGd"])
            act(U_, A_, AF.Exp, ["A", "MGn"], ["U"], bias=MG[:, 2:3])
            act(E1, Bn, AF.Exp, ["Bn", "MGn"], ["EN"], bias=MG[:, 2:3])
            act(sm[:, 1:2], MG[:, 3:4], AF.Exp, ["MGd"], ["dec"])
            tr(ps[5][0:128, 0:64], U_, ident_f[0:64, 0:64], ["U"], [("ps", 5)])
            cp("dve", Utok, ps[5][:, 0:64], [("ps", 5)], ["Utok"])
            tr(ps[5][0:128, 64:128], E1, ident_f[0:64, 0:64], ["EN"], [("ps", 5)])
            cp("dve", ENtok, ps[5][:, 64:128], [("ps", 5)], ["ENtok"])
            tr(ps[3][0:1, 64:128], sm[:, 1:2], ident_f[0:64, 0:64], ["dec"], [("ps", 3)])
            cp("dve", rrow[:, 0, :], ps[3][0:1, 64:128], [("ps", 3)], ["rrow0"])
            mm(ps[2][:, 64:128], ones_f[0:1, :], rrow[:, 0, :], True, True, r=["ones_f", "rrow0"], w=[("ps", 2)])
            cp("dve", DEC, ps[2][:, 64:128], [("ps", 2)], ["DEC"])

            def stageA1(c):
                p_ = c % 2
                hT = hT2[p_]
                hTr = ("hT", p_)
                for k in range(8):
                    tr(psb[0][:, k, :], hb2[p_][:, k * 128:(k + 1) * 128], ident_b[:], [("hb", p_)], [("ps", 0)])
                cp("act", hT, psb[0], [("ps", 0)], [hTr])
                if c + 2 < NT:
                    norm_tile(c + 2, out_bf=hb2[p_], res_bf=("hb", p_))
                for h in range(4):
                    for k in range(8):
                        mm(ps[1][:, h * 128:(h + 1) * 128], Wq[:, k, h * 128:(h + 1) * 128], hT[:, k, :],
                           k == 0, k == 7, r=["Wq", hTr], w=[("ps", 1)])
                act(qT2[p_], ps[1].rearrange("p (h t) -> p h t", h=4), AF.Copy, [("ps", 1)], [("qT", p_)],
                    scale=float(128 ** -0.5))
                for h in range(4):
                    for k in range(8):
                        mm(ps[2][:, h * 128:(h + 1) * 128], Wk[:, k, h * 128:(h + 1) * 128], hT[:, k, :],
                           k == 0, k == 7, r=["Wk", hTr], w=[("ps", 2)])
                cp("dve", kT2[p_], ps[2].rearrange("p (h t) -> p h t", h=4), [("ps", 2)], [("kT", p_)])

            def stageA2(c):
                p_ = c % 2
                hT = hT2[p_]
                hTr = ("hT", p_)
                for k in range(8):
                    mm(ps[1], hT[:, k, :], Wk[:, k, :], k == 0, k == 7, r=["Wk", hTr], w=[("ps", 1)])
                cp("act", ktok2[p_], ps[1], [("ps", 1)], [("ktok", p_)])
                for hh in range(2):
                    b = 2 - hh
                    for k in range(8):
                        mm(ps[b], hT[:, k, :], Wv[:, k, hh * 512:(hh + 1) * 512], k == 0, k == 7,
                           r=["Wv", hTr], w=[("ps", b)])
                    cp("act", vaug2[p_][:, 2 * hh:2 * hh + 2, 0:256], ps[b].rearrange("p (h v) -> p h v", h=2),
                       [("ps", b)], [("vaug", p_)])
                for hh in range(2):
                    b = 2 - hh
                    for k in range(8):
                        mm(ps[b], hT[:, k, :], Wo[:, k, hh * 512:(hh + 1) * 512], k == 0, k == 7,
                           r=["Wo", hTr], w=[("ps", b)])
                    gsl = Gs2[p_][:, hh * 512:(hh + 1) * 512]
                    act(gsl, ps[b], AF.Sigmoid, [("ps", b)], [("Gs", p_, hh)])
                    tt("pool", gsl, gsl, mng[:, hh * 512:(hh + 1) * 512], ALU.mult, [("Gs", p_, hh), "mng"],
                       [("Gs", p_, hh)])
                tt("pool", uv2[p_][:, :, 0:NVA], vaug2[p_][:, :, 0:NVA],
                   Utok[:, c:64:16].unsqueeze(2).to_broadcast([128, 4, NVA]), ALU.mult,
                   [("vaug", p_), "Utok"], [("uv", p_)])

            def stageB1(c):
                p_ = c % 2
                for h in range(4):
                    mm(ps[3][:, h * 128:(h + 1) * 128], kT2[p_][:, h, :], qT2[p_][:, h, :], True, True,
                       r=[("kT", p_), ("qT", p_)], w=[("ps", 3)])
                tt("dve", STb, ps[3].rearrange("p (h t) -> p h t", h=4),
                   maskT[:].unsqueeze(1).to_broadcast([128, 4, 128]), ALU.mult, [("ps", 3), "maskT"], ["STb"])

            def stageB2(c):
                p_ = c % 2
                qT, ktok, uv = qT2[p_], ktok2[p_], uv2[p_]
                for h in range(4):
                    bn = 4 + h
                    mm(ps[bn][:, 0:NVA], STb[:, h, :], uv[:, h, 0:NVA], True, c == 0,
                       r=["STb", ("uv", p_)], w=[("ps", bn)])
                    if c > 0:
                        mm(ps[bn][:, 0:NVA], qT[:, h, :], Cbf[:, h, 0:NVA], False, True,
                           r=[("qT", p_), ("Cbf", h)], w=[("ps", bn)])
                if c < NT - 1:
                    for h in range(4):
                        bp = 1 + (h % 2)
                        col = h * 16 + c
                        mm(ps[bp][:, 0:NVA], ktok[:, h * 128:(h + 1) * 128], uv[:, h, 0:NVA], True, True,
                           r=[("ktok", p_), ("uv", p_)], w=[("ps", bp)])
                        if c == 0:
                            ts("dve", C32[:, h, 0:NVA], ps[bp][:, 0:NVA], DEC[:, col:col + 1], ALU.mult,
                               [("ps", bp), "DEC"], [("C32", h)])
                        else:
                            tt("dve", C32[:, h, 0:NVA], ps[bp][:, 0:NVA], C32[:, h, 0:NVA], ALU.add,
                               [("ps", bp), ("C32", h)], [("C32", h)])
                            act(C32[:, h, 0:NVA], C32[:, h, 0:NVA], AF.Copy, [("C32", h), "DEC"], [("C32", h)],
                                scale=DEC[:, col:col + 1])
                        cp("pool", Cbf[:, h, 0:NVA], C32[:, h, 0:NVA], [("C32", h)], [("Cbf", h)])

            def stageB3(c):
                p_ = c % 2
                Gs = Gs2[p_]
                nres = [("ps", 4 + h) for h in range(4)]
                den4 = psall[:, 4:8, 256]
                cp("dve", s4[:, 4, :], den4, nres, ["s4_4"])
                stt(s4[:, 7, :], s4[:, 4, :], -1.0, s4[:, 4, :], ALU.mult, ALU.max, ["s4_4"], ["s4_7"])
                tt("dve", s4[:, 0, :], s4[:, 7, :], ENtok[:, c:64:16], ALU.max, ["s4_7", "ENtok"], ["s4_0"])
                recip(s4[:, 1, :], s4[:, 0, :], ["s4_0"], ["s4_1"])
                for h in range(4):
                    act(junk[:, 0:256], ps[4 + h][:, 0:256], AF.Square, [("ps", 4 + h), "s4_1"],
                        ["junk", ("s4_2", h)], scale=s4[:, 1, h:h + 1], accum=s4[:, 2, h:h + 1])
                ts("dve", s4[:, 3, :], s4[:, 2, :], 1.0 / 256, ALU.mult, [("s4_2", h) for h in range(4)], ["s4_3"],
                   s2=EPS, op1=ALU.add)
                tt("pool", s4[:, 5, :], s4[:, 3, :], negh[:], ALU.pow, ["s4_3", "negh"], ["s4_5"])
                tt("dve", s4[:, 6, :], s4[:, 5, :], s4[:, 1, :], ALU.mult, ["s4_5", "s4_1"], ["s4_6"])
                for h in range(4):
                    stt(ybf[:, h * 256:(h + 1) * 256], ps[4 + h][:, 0:256], s4[:, 6, h:h + 1],
                        Gs[:, h * 256:(h + 1) * 256], ALU.mult, ALU.mult,
                        [("ps", 4 + h), "s4_6", ("Gs", p_, h // 2)], ["ybf"])

            def stageB4(c):
                for k in range(8):
                    tr(psb[0][:, k, :], ybf[:, k * 128:(k + 1) * 128], ident_b[:], ["ybf"], [("ps", 0)])
                cp("act", yT, psb[0], [("ps", 0)], ["yT"])
                for half in range(2):
                    b = 3 if half == 0 else 0
                    for k in range(8):
                        mm(ps[b], yT[:, k, :], Wout[:, k, half * 512:(half + 1) * 512], k == 0, k == 7,
                           r=["yT", "Wout"], w=[("ps", b)])
                    add_to_x(c, half, ps[b], ("ps", b))

            if j == 0:
                Xdv = Xd.rearrange("(a p r) (h c) -> a p (r h) c", p=128, r=8, h=2)
                for a_ in range(16):
                    P.add("sp", lambda e, a_=a_: e.dma_start(
                        out=Xdv[a_], in_=ztile.unsqueeze(1).to_broadcast([128, 16, 512])),
                        r=(), w=[("Xdz", a_)], dma="xdz")
            norm_tile(0, out_bf=hb2[0], res_bf=("hb", 0))
            norm_tile(1, out_bf=hb2[1], res_bf=("hb", 1))
            stageA1(0)
            stageA2(0)
            for c in range(NT):
                stageB1(c)
                if c + 1 < NT:
                    stageA1(c + 1)
                stageB2(c)
                stageB3(c)
                if c + 1 < NT:
                    stageA2(c + 1)
                stageB4(c)
            P.barrier()

        def sconv(j):
            ar.reset()
            Win = ar.alloc([128, 8, 3 * D], BF16)
            Wout = ar.alloc([128, 8, D], BF16)
            cw = ar.alloc([128, 8, 3], F32)
            hTf2 = [ar.alloc([128, 8, 512], BF16) for _ in range(2)]
            hb2 = [hbf[:], ar.alloc([128, D], BF16)]
            U = ar.alloc([128, 8, 516], F32)
            xbs2 = [ar.alloc([128, 512], F32) for _ in range(2)]
            Y2 = [ar.alloc([128, 512], F32) for _ in range(2)]
            zT = ar.alloc([128, 8, 512], BF16)
            for hf_ in range(2):
                for i3 in range(3):
                    c0 = i3 * D + hf_ * 512
                    dma("pool", Win[:, :, c0:c0 + 512],
                        swin_d[j, :, c0:c0 + 512].rearrange("(k p) n -> p k n", p=128), f"w{i3}",
                        r=(), w=[("Win", i3, hf_)])
            dma("pool", Wout, swout_d[j].rearrange("(k p) n -> p k n", p=128), "w3", r=(), w=["Wout"])
            dma("sp", cw, scw_d[j].rearrange("p (c t) -> p c t", t=3), "sm0", r=(), w=["cw"])
            memset("pool", U[:, :, 0:2], 0.0, [("U", f) for f in range(8)])

            def convA(n):
                p_ = n % 2
                for t4 in range(4):
                    t = n * 4 + t4
                    q_ = t4 % 2
                    norm_tile(t, out_bf=hb2[q_], res_bf=("hb", q_))
                    for k in range(8):
                        tr(psb[7][:, k, :], hb2[q_][:, k * 128:(k + 1) * 128], ident_b[:], [("hb", q_)], [("ps", 7)])
                    cp("act", hTf2[p_][:, :, t4 * 128:(t4 + 1) * 128], psb[7], [("ps", 7)], [("hTf", p_)])

            def convB(n):
                p_ = n % 2
                hTf = hTf2[p_]
                for f in range(8):
                    q_ = f % 2
                    bs = q_ * 3
                    xbs, Y = xbs2[q_], Y2[q_]
                    for i3 in (2, 1, 0):
                        for k in range(8):
                            mm(ps[bs + i3], Win[:, k, i3 * D + f * 128:i3 * D + (f + 1) * 128], hTf[:, k, :],
                               k == 0, k == 7, r=[("Win", i3, f // 4), ("hTf", p_)], w=[("ps", bs + i3)])
                    cp("act", xbs, ps[bs + 2], [("ps", bs + 2)], [("xbs", q_)])
                    tt("dve", U[:, f, 2:514], ps[bs + 1], xbs, ALU.mult, [("ps", bs + 1), ("xbs", q_)], [("U", f)])
                    ts("dve", Y, U[:, f, 2:514], cw[:, f, 2:3], ALU.mult, [("U", f), "cw"], [("Y", q_)])
                    stt(Y, U[:, f, 1:513], cw[:, f, 1:2], Y, ALU.mult, ALU.add, [("U", f), "cw", ("Y", q_)], [("Y", q_)])
                    stt(Y, U[:, f, 0:512], cw[:, f, 0:1], Y, ALU.mult, ALU.add, [("U", f), "cw", ("Y", q_)], [("Y", q_)])
                    tt("dve", zT[:, f, :], ps[bs], Y, ALU.mult, [("ps", bs), ("Y", q_)], ["zT"])
                    cp("pool", U[:, f, 0:2], U[:, f, 512:514], [("U", f)], [("U", f)])
                for t4 in range(4):
                    t = n * 4 + t4
                    for half in range(2):
                        b = 6 + half
                        for k in range(8):
                            mm(ps[b], zT[:, k, t4 * 128:(t4 + 1) * 128], Wout[:, k, half * 512:(half + 1) * 512],
                               k == 0, k == 7, r=["zT", "Wout"], w=[("ps", b)])
                        add_to_x(t, half, ps[b], ("ps", b))

            convA(0)
            for n in range(4):
                if n + 1 < 4:
                    convA(n + 1)
                convB(n)
            P.barrier()

        def moe(i, next_sub=None):
            ar.reset()
            DEST = ar.alloc([128, NT, 2], I32)
            WTS = ar.alloc([128, NT, 2], F32)
            mark = ar.off
            Wg = [ar.alloc([128, 8, 512], BF16) for _ in range(2)]
            Wu = [ar.alloc([128, 8, 512], BF16) for _ in range(2)]
            Wd = [ar.alloc([128, 4, D], BF16) for _ in range(2)]
            wend = ar.off
            ar.off = mark
            Wr = ar.alloc([128, 8, 36], F32)
            rb = ar.alloc([128, 36], F32)
            hfs = [ar.alloc([128, D], F32) for _ in range(2)]
            hTfs = [ar.alloc([128, 8, 128], F32) for _ in range(2)]
            ar_small_end = ar.off
            ar.off = max(ar.off, wend)
            HB = ar.alloc([128, NT, D], BF16)
            ar.off = ar_small_end
            LA = ar.alloc([128, NT, 36], F32)
            G4 = [ar.alloc([128, NT, 4], F32) for _ in range(4)]
            Lm = ar.alloc([128, NT, 32], F32)
            Lm2 = ar.alloc([128, NT, 32], F32)
            M1 = ar.alloc([128, NT, 32], F32)
            M2 = ar.alloc([128, NT, 32], F32)
            E = ar.alloc([128, NT, 32], F32)
            PRE = ar.alloc([128, NT, 32], F32)
            CNT = ar.alloc([128, NT, 32], F32)
            JK = ar.alloc([128, NT, 32], F32)
            SV = ar.alloc([128, 28, NT], F32)
            assert ar.off <= wend, ("routing scratch overruns weight slots", ar.off, wend)

            dma("sp", Wr, rw_d[i].rearrange("(k p) n -> p k n", p=128), "sm0", r=(), w=["Wr"])
            dma("sp", rb, rb_d[i, :].partition_broadcast(128), "sm1", r=(), w=["rb"])

            def load_expert(e):
                s_ = e % 2
                g = i * NE + e
                P.add("pool", lambda e, s_=s_, g=g: e.dma_start(
                    out=Wg[s_], in_=ewg_d[g].rearrange("(p k) n -> p k n", p=128), max_dma_last_dim=8192),
                    r=(), w=[("Wg", s_)], dma=f"w{s_}")
                P.add("pool", lambda e, s_=s_, g=g: e.dma_start(
                    out=Wu[s_], in_=ewu_d[g].rearrange("(p k) n -> p k n", p=128), max_dma_last_dim=8192),
                    r=(), w=[("Wu", s_)], dma=f"w{2 + s_}")
                dma("pool", Wd[s_], ewd_d[g].rearrange("(k p) n -> p k n", p=128), f"w{4 + s_}", r=(), w=[("Wd", s_)])

            def r1_norm(t):
                p_ = t % 2
                norm_tile(t, out_f=hfs[p_], res_f=("hf", p_))
                cp("pool", HB[:, t, :].rearrange("q (k p) -> q p k", p=128),
                   hfs[p_].rearrange("q (p k) -> q p k", k=8), [("hf", p_)], [("HB", t)])

            def r1_router(t):
                p_ = t % 2
                hf = hfs[p_]
                hTf = hTfs[p_]
                for g4 in range(2):
                    bk = p_ * 2 + g4
                    for k4 in range(4):
                        k = g4 * 4 + k4
                        tr(ps[bk][:, k4 * 128:(k4 + 1) * 128], hf[:, k * 128:(k + 1) * 128], ident_f[:],
                           [("hf", p_)], [("ps", bk)])
                    cp("act", hTf[:, g4 * 4:g4 * 4 + 4, :],
                       ps[bk].rearrange("p (k m) -> p k m", k=4), [("ps", bk)], [("hTf", p_, g4)])
                bl = 4 + p_
                for k in range(8):
                    mm(ps[bl][:, 0:36], hTf[:, k, :], Wr[:, k, :], k == 0, k == 7,
                       r=[("hTf", p_, k // 4), "Wr"], w=[("ps", bl)])
                tt("dve", LA[:, t, :], ps[bl][:, 0:36], rb, ALU.add, [("ps", bl), "rb"], ["LA"])

            r1_norm(0)
            for t in range(NT):
                if t + 1 < NT:
                    r1_norm(t + 1)
                r1_router(t)

            def bc(v, n):
                return v.unsqueeze(2).to_broadcast([128, NT, n])

            def red(out, in_, op, r, w):
                P.add("dve", lambda e: e.tensor_reduce(out=out, in_=in_, axis=AX.X, op=op), r=r, w=w)

            LA4 = LA[:, :, 0:4]
            LAE = LA[:, :, 4:36]
            red(SV[:, 0, :], LA4, ALU.max, ["LA"], ["gmax"])
            tt("dve", G4[0], LA4, bc(SV[:, 0, :], 4), ALU.subtract, ["LA", "gmax"], ["G40"])
            act(G4[1], G4[0], AF.Exp, ["G40"], ["G41"])
            red(SV[:, 1, :], G4[1], ALU.add, ["G41"], ["gsum"])
            recip(SV[:, 2, :], SV[:, 1, :], ["gsum"], ["psel"])
            tt("dve", G4[2], LA4, bc(SV[:, 0, :], 4), ALU.is_equal, ["LA", "gmax"], ["G42"])
            ts("dve", G4[3], G4[2], -1.0, ALU.add, ["G42"], ["G43"], s2=1.0e30, op1=ALU.mult)
            tt("dve", Lm.rearrange("p t (g e) -> p t g e", g=4), LAE.rearrange("p t (g e) -> p t g e", g=4),
               G4[3].unsqueeze(3).to_broadcast([128, NT, 4, 8]), ALU.add, ["LA", "G43"], ["Lm"])
            red(SV[:, 3, :], Lm, ALU.max, ["Lm"], ["m1"])
            tt("dve", M1, Lm, bc(SV[:, 3, :], 32), ALU.is_equal, ["Lm", "m1"], ["M1"])
            stt(Lm2, M1, -1.0e30, Lm, ALU.mult, ALU.add, ["M1", "Lm"], ["Lm2"])
            red(SV[:, 4, :], Lm2, ALU.max, ["Lm2"], ["m2"])
            tt("dve", M2, Lm2, bc(SV[:, 4, :], 32), ALU.is_equal, ["Lm2", "m2"], ["M2"])
            iob = iota_f[:].unsqueeze(1).to_broadcast([128, NT, 32])
            tt("dve", JK, M1, iob, ALU.mult, ["M1", "iota_f"], ["JK"])
            red(SV[:, 5, :], JK, ALU.add, ["JK"], ["eid0"])
            tt("dve", JK, M2, iob, ALU.mult, ["M2", "iota_f"], ["JK"])
            red(SV[:, 6, :], JK, ALU.add, ["JK"], ["eid1"])
            tt("dve", SV[:, 7, :], SV[:, 4, :], SV[:, 3, :], ALU.subtract, ["m1", "m2"], ["d"])
            act(SV[:, 8, :], SV[:, 7, :], AF.Exp, ["d"], ["e"])
            ts("dve", SV[:, 9, :], SV[:, 8, :], 1.0, ALU.add, ["e"], ["e1"])
            recip(SV[:, 10, :], SV[:, 9, :], ["e1"], ["r"])
            tt("dve", SV[:, 11, :], SV[:, 10, :], SV[:, 2, :], ALU.mult, ["r", "psel"], ["w0"])
            tt("dve", SV[:, 12, :], SV[:, 2, :], SV[:, 11, :], ALU.subtract, ["psel", "w0"], ["w1"])
            tt("dve", E, M1, M2, ALU.add, ["M1", "M2"], ["E"])
            Ef = E.rearrange("p t e -> p (t e)")
            mm(ps[6], Lt[:], Ef, True, True, r=["Lt", "E"], w=[("ps", 6)])
            mm(ps[7], ones_f[:], Ef, True, True, r=["ones_f", "E"], w=[("ps", 7)])
            memset("dve", PRE[:, 0, :], 0.0, ["PRE"])
            for t in range(1, NT):
                tt("dve", PRE[:, t, :], PRE[:, t - 1, :], ps[7][:, (t - 1) * 32:t * 32], ALU.add,
                   ["PRE", ("ps", 7)], ["PRE"])
            tt("dve", CNT.rearrange("p t e -> p (t e)"), PRE.rearrange("p t e -> p (t e)"), ps[6], ALU.add,
               ["PRE", ("ps", 6)], ["CNT"])
            for kk, MM in ((0, M1), (1, M2)):
                tt("dve", JK, MM, CNT, ALU.mult, ["M1", "M2", "CNT"], ["JK"])
                red(SV[:, 13 + kk, :], JK, ALU.add, ["JK"], [("pos", kk)])
                stt(SV[:, 15 + kk, :], SV[:, 5 + kk, :], float(CAP), SV[:, 13 + kk, :], ALU.mult, ALU.add,
                    ["eid0", "eid1", ("pos", kk)], [("dst", kk)])
                ts("dve", SV[:, 17 + kk, :], SV[:, 13 + kk, :], float(CAP), ALU.is_ge, [("pos", kk)], [("ov", kk)],
                   s2=1.0e6, op1=ALU.mult)
                tt("dve", SV[:, 15 + kk, :], SV[:, 15 + kk, :], SV[:, 17 + kk, :], ALU.add,
                   [("dst", kk), ("ov", kk)], [("dst", kk)])
                cp("dve", DEST[:, :, kk], SV[:, 15 + kk, :], [("dst", kk)], ["DEST"])
                ts("dve", SV[:, 19 + kk, :], SV[:, 13 + kk, :], float(CAP), ALU.is_lt, [("pos", kk)], [("keep", kk)])
                tt("dve", WTS[:, :, kk], SV[:, 11 + kk, :], SV[:, 19 + kk, :], ALU.mult,
                   ["w0", "w1", ("keep", kk)], ["WTS"])

            P._merge(P.pending["pool"], ("d", "xdz", P.dma_cnt["xdz"]), "raw")
            for t in range(NT):
                for kk in range(2):
                    P.add("pool", lambda e, t=t, kk=kk: e.indirect_dma_start(
                        out=Xd, out_offset=bass.IndirectOffsetOnAxis(ap=DEST[:, t, kk:kk + 1], axis=0),
                        in_=HB[:, t, :], in_offset=None, bounds_check=breg[0], oob_is_err=False),
                        r=["DEST", ("HB", t)], w=[("Xd", t, kk)], dma=f"sc{kk}")
            P.barrier()

            ar.off = wend
            Xtok = [ar.alloc([128, NJ, D], BF16) for _ in range(2)]
            XeT = [ar.alloc([128, 8, CAP], BF16) for _ in range(2)]
            sg = [ar.alloc([128, CAP], F32) for _ in range(2)]
            AT = [ar.alloc([128, 4, CAP], BF16) for _ in range(2)]
            yo_lo = ar.off
            Yo = [ar.alloc([128, D], BF16) for _ in range(4)]

            def x_load(e):
                s_ = e % 2
                dma("sp", Xtok[s_], Xd[e * CAP:(e + 1) * CAP, :].rearrange("(j p) d -> p j d", p=128), f"xl{s_}",
                    r=(), w=[("Xtok", s_)])

            def t_phase(e):
                s_ = e % 2
                for jj in range(NJ):
                    b = jj % 2
                    for k in range(8):
                        tr(psb[b][:, k, :], Xtok[s_][:, jj, k * 128:(k + 1) * 128], ident_b[:],
                           [("Xtok", s_)], [("ps", b)])
                    cp("act" if jj % 2 == 0 else "dve", XeT[s_][:, :, jj * 128:(jj + 1) * 128], psb[b],
                       [("ps", b)], [("XeT", s_, jj)])

            def gu_phase(e):
                s_ = e % 2
                xr = [("XeT", s_, jj) for jj in range(NJ)]
                for f in range(4):
                    bg = 2 + (f % 2) * 2
                    bu = bg + 1
                    for k in range(8):
                        mm(ps[bg], Wg[s_][:, k, f * 128:(f + 1) * 128], XeT[s_][:, k, :], k == 0, k == 7,
                           r=[("Wg", s_)] + xr, w=[("ps", bg)])
                    for k in range(8):
                        mm(ps[bu], Wu[s_][:, k, f * 128:(f + 1) * 128], XeT[s_][:, k, :], k == 0, k == 7,
                           r=[("Wu", s_)] + xr, w=[("ps", bu)])
                    act(sg[f % 2], ps[bg], AF.Silu, [("ps", bg)], [("sg", f % 2)])
                    tt("dve", AT[s_][:, f, :], ps[bu], sg[f % 2], ALU.mult, [("ps", bu), ("sg", f % 2)],
                       [("AT", s_, f)])

            yo_ctr = [0]

            def d_phase(e):
                s_ = e % 2
                ar_ = [("AT", s_, f) for f in range(4)]
                for jj in range(NJ):
                    yi = yo_ctr[0] % 4
                    yo_ctr[0] += 1
                    yo = Yo[yi]
                    yor = ("Yo", yi)
                    for half in range(2):
                        b = 6 + half
                        for f in range(4):
                            mm(ps[b], AT[s_][:, f, jj * 128:(jj + 1) * 128], Wd[s_][:, f, half * 512:(half + 1) * 512],
                               f == 0, f == 3, r=[("Wd", s_)] + ar_, w=[("ps", b)])
                        tt("dve", yo[:, half * 512:(half + 1) * 512], ps[b], modG[:, half * 512:(half + 1) * 512],
                           ALU.mult, [("ps", b), "modG"], [yor])
                    dma("sp", Yd[e * CAP + jj * 128:e * CAP + (jj + 1) * 128, :], yo, f"yo{yi}",
                        r=[yor], w=[("Yd", e, jj)])

            x_load(0)
            x_load(1)
            load_expert(0)
            t_phase(0)
            for e_ in range(NE):
                if e_ + 2 < NE:
                    x_load(e_ + 2)
                if e_ + 1 < NE:
                    load_expert(e_ + 1)
                gu_phase(e_)
                if e_ + 1 < NE:
                    t_phase(e_ + 1)
                d_phase(e_)
            P.barrier()

            nxt = None
            if next_sub is not None:
                yo_off = ar.off
                nxt = adaln_issue(next_sub, ng_d[next_sub, :], base=mark)
                assert ar.off <= yo_lo, ("adaLN prefetch overlaps live combine buffers", ar.off, yo_lo)
                ar.off = yo_off
            for yi in range(4):
                memset("pool", Yo[yi], 0.0, [("Yo", yi)])

            def gathers(t):
                for kk in range(2):
                    yi = (t % 2) * 2 + kk
                    P.add("pool", lambda e, t=t, kk=kk, yi=yi: e.indirect_dma_start(
                        out=Yo[yi][:], out_offset=None, in_=Yd,
                        in_offset=bass.IndirectOffsetOnAxis(ap=DEST[:, t, kk:kk + 1], axis=0),
                        bounds_check=breg[0], oob_is_err=False),
                        r=["DEST"], w=[("Yo", yi)], dma=f"ga{yi}")

            gathers(0)
            for t in range(NT):
                if t + 1 < NT:
                    gathers(t + 1)
                y0, y1 = (t % 2) * 2, (t % 2) * 2 + 1
                stt(xres[:, t, :], Yo[y0], WTS[:, t, 0:1], xres[:, t, :], ALU.mult, ALU.add,
                    [("Yo", y0), "WTS", ("x", t)], [("x", t)])
                stt(xres[:, t, :], Yo[y1], WTS[:, t, 1:2], xres[:, t, :], ALU.mult, ALU.add,
                    [("Yo", y1), "WTS", ("x", t)], [("x", t)])
            if nxt is not None:
                adaln_finish(nxt)
                return
            P.barrier()

        for t4 in range(4):
            dma("sp", xres[:, t4 * 4:(t4 + 1) * 4, :],
                x_d[t4 * 512:(t4 + 1) * 512, :].rearrange("(t p) d -> p t d", p=128),
                "xin", r=(), w=[("x", t) for t in range(t4 * 4, t4 * 4 + 4)])
        dma("sp", cs[:], c_d, "cin", r=(), w=["cs"])
        pre0 = [None]
        breg = [None]

        def _mk_breg(e):
            breg[0] = e.to_reg(NE * CAP - 1)
            return e.memset(ident_f[:], 0.0)
        P.add("pool", _mk_breg, r=(), w=["ident_f"])
        P.add("pool", lambda e: e.affine_select(out=ident_f[:], in_=ident_f[:], pattern=[[-1, 128]], base=0,
                                                channel_multiplier=1, compare_op=ALU.not_equal, fill=1.0),
              r=["ident_f"], w=["ident_f"])
        cp("pool", ident_b[:], ident_f[:], ["ident_f"], ["ident_b"])
        memset("pool", maskT[:], 1.0, ["maskT"])
        P.add("pool", lambda e: e.affine_select(out=maskT[:], in_=maskT[:], pattern=[[1, 128]], base=0,
                                                channel_multiplier=-1, compare_op=ALU.is_ge, fill=0.0),
              r=["maskT"], w=["maskT"])
        memset("pool", Lt[:], 1.0, ["Lt"])
        P.add("pool", lambda e: e.affine_select(out=Lt[:], in_=Lt[:], pattern=[[1, 128]], base=-1,
                                                channel_multiplier=-1, compare_op=ALU.is_ge, fill=0.0),
              r=["Lt"], w=["Lt"])
        memset("pool", ones_f[:], 1.0, ["ones_f"])
        memset("pool", TT[:], 1.0, ["TT"])
        P.add("pool", lambda e: e.affine_select(out=TT[:], in_=TT[:], pattern=[[1, 64]], base=-1,
                                                channel_multiplier=-1, compare_op=ALU.is_ge, fill=0.0),
              r=["TT"], w=["TT"])
        for hb in range(1, 4):
            P.add("pool", lambda e, hb=hb: e.affine_select(out=TT[:, 16 * hb:16 * hb + 16],
                                                           in_=TT[:, 16 * hb:16 * hb + 16],
                                                           pattern=[[0, 16]], base=-16 * hb, channel_multiplier=1,
                                                           compare_op=ALU.is_ge, fill=0.0),
                  r=["TT"], w=["TT"])
        P.add("pool", lambda e: e.iota(iota_i[:], pattern=[[1, 32]], base=0, channel_multiplier=0),
              r=(), w=["iota_i"])
        cp("pool", iota_f[:], iota_i[:], ["iota_i"], ["iota_f"])
        act(sc[:], cs[:], AF.Silu, ["cs"], ["sc"])
        memset("pool", negh[:], -0.5, ["negh"])
        pre0[0] = adaln_issue(0, ng_d[0, :])
        P.bg_keys.add("xdz")
        memset("pool", ztile, 0.0, ["ztile"])
        P.barrier()


        pre_done = False
        for sub in range(n_sub):
            i, s_ = sub // 2, sub % 2
            if not pre_done:
                if sub == 0:
                    adaln_finish(pre0[0])
                else:
                    adaln(sub, ng_d[sub, :])
            pre_done = False
            if s_ == 0:
                if i % 2 == 0:
                    mlstm(i // 2)
                else:
                    sconv(i // 2)
            else:
                if sub + 1 < n_sub:
                    moe(i, next_sub=sub + 1)
                    pre_done = True
                else:
                    moe(i)

        ar.reset()
        if do_final:
            dma("sp", modA[:], fg_d[0, :].partition_broadcast(128), "gbc", r=(), w=["modA"])
            compute_rstd()
        outs = [ar.alloc([128, D], F32) for _ in range(2)]
        for t in range(NT):
            o = outs[t % 2]
            if do_final:
                norm_tile(t, out_f=o, shift=False)
                dma("sp", y_d[t * 128:(t + 1) * 128, :], o, f"out{t % 2}", r=["hf"], w=[("y", t)])
            else:
                dma("sp", y_d[t * 128:(t + 1) * 128, :], xres[:, t, :], f"out{t % 2}", r=[("x", t)], w=[("y", t)])

        dma_keys = sorted(P.dma_cnt.keys())
        eng_sems = {e: es.enter_context(nc.semaphore(f"s_{e}")) for e in ENGS}
        dma_sems = {k: es.enter_context(nc.semaphore(f"d_{k}")) for k in dma_keys}
        bodies = P.emit(None, eng_sems, dma_sems)
        with nc.Block() as block:
            block.tensor(bodies["pe"])
            block.scalar(bodies["act"])
            block.vector(bodies["dve"])
            block.gpsimd(bodies["pool"])
            block.sync(bodies["sp"])
        n_ops = {e: len(P.ops[e]) for e in ENGS}
    return nc, n_ops


def make_in_maps(inputs):
    f = lambda a: np.ascontiguousarray(np.asarray(a, dtype=np.float32))
    x = f(inputs["x"])
    c = f(inputs["c"])
    B = x.shape[0]
    shared = {
        "ada_w": f(inputs["ada_w"]).reshape(2 * DEPTH, D, 3 * D),
        "ada_b": f(inputs["ada_b"]).reshape(2 * DEPTH, 3 * D),
        "norm_g": f(inputs["norm_g"]).reshape(2 * DEPTH, D),
        "final_g": f(inputs["final_g"]).reshape(1, D),
        "m_w_in": f(inputs["m_w_in"]),
        "m_b_gates": f(inputs["m_b_gates"]),
        "m_norm_g": f(inputs["m_norm_g"]),
        "m_w_out": f(inputs["m_w_out"]),
        "s_w_in": f(inputs["s_w_in"]),
        "s_conv_w": np.ascontiguousarray(
            f(inputs["s_conv_w"]).reshape(2, 3, 8, 128).transpose(0, 3, 2, 1).reshape(2, 128, 24)),
        "s_w_out": f(inputs["s_w_out"]),
        "r_w": np.ascontiguousarray(np.concatenate([f(inputs["r_w_group"]), f(inputs["r_w_expert"])], axis=-1)),
        "r_b": np.ascontiguousarray(np.concatenate([f(inputs["r_b_group"]), f(inputs["r_b_expert"])], axis=-1)),
        "e_w_gate": f(inputs["e_w_gate"]).reshape(DEPTH * NE, D, 512),
        "e_w_up": f(inputs["e_w_up"]).reshape(DEPTH * NE, D, 512),
        "e_w_down": f(inputs["e_w_down"]).reshape(DEPTH * NE, 512, D),
    }
    maps = []
    for b in range(B):
        m = dict(shared)
        m["x"] = np.ascontiguousarray(x[b])
        m["c"] = np.ascontiguousarray(c[b].reshape(8, 128).T)
        maps.append(m)
    return maps


def kernel(**inputs):
    nc, _ = build_program()
    in_maps = make_in_maps(inputs)
    res = run_bass_kernel_spmd(nc, in_maps, core_ids=list(range(len(in_maps))))
    return np.stack([r["y"] for r in res.results], axis=0).astype(np.float32)
```
